# Optimizing a Trainium2 kernel written in Bass

```python
import jax, jax.numpy as jnp
from jax import lax
import numpy as np

D_MODEL = 1024
BATCH = 4
SEQ = 8192
DEPTH = 1

CTX_LEN = 256
GRID_W = 64
MIX_WIDTH = D_MODEL
FOURIER_WIDTH = MIX_WIDTH // 2
LRU_WIDTH = MIX_WIDTH // 2
FOURIER_HEADS = 8
FOURIER_HEAD_DIM = FOURIER_WIDTH // FOURIER_HEADS
LRU_HEADS = 8
LRU_HEAD_DIM = LRU_WIDTH // LRU_HEADS
IN_WIDTH = FOURIER_WIDTH + 2 * LRU_WIDTH
CONV_W = 4
LRU_C = 8.0
PEER_HEADS = 8
PEER_NKEYS = 128
PEER_EXPERTS = PEER_NKEYS * PEER_NKEYS
PEER_KEY_HALF = 128
PEER_TOPK = 16
PEER_CHUNK = 128
N_MOD = 6
EPS = 1e-6

kernel_name = 'hybrid_fnet_rglru_peer_block'


def rmsnorm(x, g):
    xf = x.astype(jnp.float32)
    y = xf * lax.rsqrt(jnp.mean(xf * xf, axis=-1, keepdims=True) + EPS)
    return (y * g.astype(jnp.float32)).astype(x.dtype)


def modulate(h, shift, scale):
    return h * (1 + scale) + shift


def dwconv(x, w, b):
    pad_l = CONV_W // 2
    pad_r = CONV_W - 1 - pad_l
    L = x.shape[-2]
    xp = jnp.pad(x, [(0, 0)] * (x.ndim - 2) + [(pad_l, pad_r), (0, 0)])
    y = b
    for k in range(CONV_W):
        y = y + xp[..., k:k + L, :] * w[k]
    return y


def fourier_mix(f):
    b_, L, _ = f.shape
    fh = f.astype(jnp.float32).reshape(b_, L, FOURIER_HEADS, FOURIER_HEAD_DIM)
    y = jnp.fft.fftn(fh, axes=(1, 3), norm='ortho').real
    return y.reshape(b_, L, FOURIER_WIDTH).astype(f.dtype)


def _lin_combine(left, right):
    a_l, b_l = left
    a_r, b_r = right
    return a_l * a_r, a_r * b_l + b_r


def rglru_scan(xc, w_a, b_a, w_x, b_x, lam, h0, reverse):
    b_, L, C = xc.shape
    xf = xc.astype(jnp.float32)
    xh = xf.reshape(b_, L, LRU_HEADS, LRU_HEAD_DIM)
    r = jax.nn.sigmoid(jnp.einsum('blhi,hij->blhj', xh, w_a.astype(jnp.float32)).reshape(b_, L, C) + b_a.astype(jnp.float32))
    i = jax.nn.sigmoid(jnp.einsum('blhi,hij->blhj', xh, w_x.astype(jnp.float32)).reshape(b_, L, C) + b_x.astype(jnp.float32))
    log_a = -LRU_C * r * jax.nn.softplus(-lam.astype(jnp.float32))
    a = jnp.exp(log_a)
    inp = jnp.sqrt(-jnp.expm1(2.0 * log_a)) * (i * xf)
    A, Bc = lax.associative_scan(_lin_combine, (a, inp), reverse=reverse, axis=1)
    h = A * h0[:, None, :] + Bc
    final = h[:, 0] if reverse else h[:, -1]
    return h, final


def peer_ffn(h, w_q, sub_keys, u_tab, v_tab):
    shp = h.shape
    flat = h.reshape(-1, PEER_CHUNK, shp[-1])

    def body(xc):
        T = xc.shape[0]
        q = (xc @ w_q).reshape(T, PEER_HEADS, 2, PEER_KEY_HALF)
        s = jnp.einsum('thpd,hpkd->thpk', q, sub_keys).astype(jnp.float32)
        s1, i1 = lax.top_k(s[:, :, 0], PEER_TOPK)
        s2, i2 = lax.top_k(s[:, :, 1], PEER_TOPK)
        cand = (s1[..., :, None] + s2[..., None, :]).reshape(T, PEER_HEADS, PEER_TOPK * PEER_TOPK)
        cidx = (i1[..., :, None] * PEER_NKEYS + i2[..., None, :]).reshape(T, PEER_HEADS, PEER_TOPK * PEER_TOPK)
        sc, pos = lax.top_k(cand, PEER_TOPK)
        idx = jnp.take_along_axis(cidx, pos, axis=-1)
        g = jax.nn.softmax(sc, axis=-1)
        u = u_tab[idx]
        act = jax.nn.gelu(jnp.einsum('thkd,td->thk', u, xc).astype(jnp.float32))
        return jnp.einsum('thk,thkd->td', (g * act).astype(xc.dtype), v_tab[idx])

    return lax.map(body, flat).reshape(shp)


def setup_inputs(seed: int = 0) -> dict:
    key = jax.random.key(seed)
    ks = jax.random.split(key, 24)
    D = D_MODEL

    def nrm(k, shape, scale):
        return jax.random.normal(k, shape, jnp.float32) * scale

    u = jax.random.uniform(ks[14], (DEPTH, 2, LRU_WIDTH), jnp.float32, minval=0.9, maxval=0.999)
    a0 = u ** (1.0 / LRU_C)
    return {
        'x': nrm(ks[0], (BATCH, SEQ, D), 1.0),
        'c': nrm(ks[1], (BATCH, D), 1.0),
        'ctx': nrm(ks[2], (BATCH, CTX_LEN, D), 1.0),
        'c_ctx': nrm(ks[3], (D,), 1.0),
        'w_mod': nrm(ks[4], (DEPTH, D, N_MOD * D), 0.5 * D ** -0.5),
        'b_mod': nrm(ks[5], (DEPTH, N_MOD * D), 0.02),
        'norm1_g': 1.0 + nrm(ks[6], (DEPTH, D), 0.02),
        'w_in': nrm(ks[7], (DEPTH, D, IN_WIDTH), D ** -0.5),
        'conv_w': nrm(ks[8], (DEPTH, CONV_W, LRU_WIDTH), CONV_W ** -0.5),
        'conv_b': nrm(ks[9], (DEPTH, LRU_WIDTH), 0.02),
        'lru_w_a': nrm(ks[10], (DEPTH, 2, LRU_HEADS, LRU_HEAD_DIM, LRU_HEAD_DIM), LRU_HEAD_DIM ** -0.5),
        'lru_b_a': nrm(ks[11], (DEPTH, 2, LRU_WIDTH), 0.02),
        'lru_w_x': nrm(ks[12], (DEPTH, 2, LRU_HEADS, LRU_HEAD_DIM, LRU_HEAD_DIM), LRU_HEAD_DIM ** -0.5),
        'lru_b_x': nrm(ks[13], (DEPTH, 2, LRU_WIDTH), 0.02),
        'lru_lambda': jnp.log(a0) - jnp.log1p(-a0),
        'fourier_out_g': 1.0 + nrm(ks[15], (DEPTH, FOURIER_WIDTH), 0.02),
        'lru_out_g': 1.0 + nrm(ks[16], (DEPTH, LRU_WIDTH), 0.02),
        'w_out': nrm(ks[17], (DEPTH, MIX_WIDTH, D), MIX_WIDTH ** -0.5),
        'norm2_g': 1.0 + nrm(ks[18], (DEPTH, D), 0.02),
        'peer_w_q': nrm(ks[19], (DEPTH, D, PEER_HEADS * 2 * PEER_KEY_HALF), D ** -0.5),
        'peer_sub_keys': nrm(ks[20], (DEPTH, PEER_HEADS, 2, PEER_NKEYS, PEER_KEY_HALF), PEER_KEY_HALF ** -0.5),
        'peer_u': nrm(ks[21], (DEPTH, PEER_EXPERTS, D), D ** -0.5),
        'peer_v': nrm(ks[22], (DEPTH, PEER_EXPERTS, D), PEER_HEADS ** -0.5),
        'final_norm_g': 1.0 + nrm(ks[23], (D,), 0.02),
    }


def reference(x, c, ctx, c_ctx, w_mod, b_mod, norm1_g, w_in, conv_w, conv_b, lru_w_a, lru_b_a,
              lru_w_x, lru_b_x, lru_lambda, fourier_out_g, lru_out_g, w_out, norm2_g,
              peer_w_q, peer_sub_keys, peer_u, peer_v, final_norm_g):
    B_, L, _ = x.shape
    ROWS = L // GRID_W
    C_LEN = ctx.shape[1]
    split_pts = [FOURIER_WIDTH, FOURIER_WIDTH + LRU_WIDTH]
    for l in range(DEPTH):
        update_ctx = l + 1 < DEPTH
        mod_x = jax.nn.silu(c) @ w_mod[l] + b_mod[l]
        mod_c = jax.nn.silu(c_ctx) @ w_mod[l] + b_mod[l]
        sh1_x, sc1_x, g1_x, sh2_x, sc2_x, g2_x = [m[:, None, :] for m in jnp.split(mod_x, N_MOD, axis=-1)]
        sh1_c, sc1_c, g1_c, sh2_c, sc2_c, g2_c = jnp.split(mod_c, N_MOD, axis=-1)

        hx = modulate(rmsnorm(x, norm1_g[l]), sh1_x, sc1_x)
        hc = modulate(rmsnorm(ctx, norm1_g[l]), sh1_c, sc1_c)
        fx, ux, gx = jnp.split(hx @ w_in[l], split_pts, axis=-1)
        fc, uc, gc = jnp.split(hc @ w_in[l], split_pts, axis=-1)

        ux = dwconv(ux.reshape(B_, ROWS, GRID_W, LRU_WIDTH), conv_w[l], conv_b[l]).reshape(B_, L, LRU_WIDTH)
        uc = dwconv(uc, conv_w[l], conv_b[l])
        h_zero = jnp.zeros((B_, LRU_WIDTH), jnp.float32)
        lat_dirs = []
        ctx_dirs = []
        for d, rev in ((0, False), (1, True)):
            hcd, fin = rglru_scan(uc, lru_w_a[l, d], lru_b_a[l, d], lru_w_x[l, d], lru_b_x[l, d],
                                  lru_lambda[l, d], h_zero, rev)
            hxd, _ = rglru_scan(ux, lru_w_a[l, d], lru_b_a[l, d], lru_w_x[l, d], lru_b_x[l, d],
                                lru_lambda[l, d], fin, rev)
            lat_dirs.append(hxd)
            ctx_dirs.append(hcd)
        rx = ((lat_dirs[0] + lat_dirs[1]) * jax.nn.gelu(gx.astype(jnp.float32))).astype(x.dtype)

        mx = jnp.concatenate([rmsnorm(fourier_mix(fx), fourier_out_g[l]),
                              rmsnorm(rx, lru_out_g[l])], axis=-1) @ w_out[l]
        x = x + g1_x * mx

        h2x = modulate(rmsnorm(x, norm2_g[l]), sh2_x, sc2_x)
        x = x + g2_x * peer_ffn(h2x, peer_w_q[l], peer_sub_keys[l], peer_u[l], peer_v[l])

        if update_ctx:
            rc = ((ctx_dirs[0] + ctx_dirs[1]) * jax.nn.gelu(gc.astype(jnp.float32))).astype(ctx.dtype)
            mc = jnp.concatenate([rmsnorm(fourier_mix(fc), fourier_out_g[l]),
                                  rmsnorm(rc, lru_out_g[l])], axis=-1) @ w_out[l]
            ctx = ctx + g1_c * mc
            h2c = modulate(rmsnorm(ctx, norm2_g[l]), sh2_c, sc2_c)
            ctx = ctx + g2_c * peer_ffn(h2c, peer_w_q[l], peer_sub_keys[l], peer_u[l], peer_v[l]).reshape(B_, C_LEN, D_MODEL)
    return rmsnorm(x, final_norm_g)
```

```python
import os
import numpy as np
import concourse.bass as bass
import concourse.mybir as mybir
from concourse.bass_utils import run_bass_kernel_spmd
from contextlib import ExitStack

F32 = mybir.dt.float32
BF16 = mybir.dt.bfloat16
U32 = mybir.dt.uint32
AF = mybir.ActivationFunctionType
ALU = mybir.AluOpType
AX = mybir.AxisListType

ENGS = ["tensor", "vector", "scalar", "gpsimd", "sync"]
EPS = 1e-6
L = 8192
CTX = 256
T = L + CTX
D = 1024
HALF = 4096
TB = 256
NCH = 128


class Buf:
    def __init__(self, t, name="", excl=False):
        self.t = t
        self.name = name
        self.excl = excl
        self.w = {}
        self.r = {}

    def __getitem__(self, idx):
        return self.t[idx]


class Sched:
    def __init__(self, nc, es):
        self.nc = nc
        self.es = es
        self.ops = {e: [] for e in ENGS}
        self.sems = {}
        for e in ["tensor", "vector", "scalar", "gpsimd"]:
            self.sems[e] = es.enter_context(nc.semaphore("c_" + e))
        self.count = {e: 0 for e in ENGS}
        self.dma_sems = {}
        self.dma_cnt = {}
        self.dma_rr = {}
        for q, n in (("sync", 32), ("gpsimd", 16), ("scalar", 8)):
            self.dma_sems[q] = [es.enter_context(nc.semaphore("d_%s%d" % (q, i))) for i in range(n)]
            self.dma_cnt[q] = [0] * n
            self.dma_rr[q] = 0
        self.waited = {}
        self.phase_dma = []
        self.cur_es = es

    def sbuf(self, name, shape, dtype):
        return Buf(self.cur_es.enter_context(self.nc.sbuf_tensor(name, list(shape), dtype)), name)

    def alias(self, buf, name=""):
        return Buf(buf.t, name)

    def psum(self, name, shape, dtype=F32):
        return Buf(self.es.enter_context(self.nc.psum_tensor(name, list(shape), dtype)), name, excl=True)

    def _deps(self, reads, writes, eng=None):
        deps = []
        for b in reads:
            deps.extend((sk, v, e) for sk, (v, e) in b.w.items())
            if b.excl:
                deps.extend((sk, v, e) for sk, (v, e) in b.r.items() if e != eng)
        for b in writes:
            deps.extend((sk, v, e) for sk, (v, e) in b.w.items())
            deps.extend((sk, v, e) for sk, (v, e) in b.r.items())
        return deps

    def _waits(self, eng, deps, skip_same_pe=True):
        waits = []
        for (sk, v, e) in deps:
            if skip_same_pe and eng == "tensor" and e == "tensor":
                continue
            key = (eng, sk)
            if self.waited.get(key, 0) >= v:
                continue
            self.waited[key] = v
            waits.append((sk, v))
        return waits

    def _semobj(self, sk):
        if isinstance(sk, tuple):
            return self.dma_sems[sk[0]][sk[1]]
        return self.sems[sk]

    def _record(self, tok, reads, writes):
        sk, v, e = tok
        for b in reads:
            if b.r.get(sk, (0, None))[0] < v:
                b.r[sk] = (v, e)
        for b in writes:
            b.w = {sk: (v, e)}
            b.r = {}

    def op(self, eng, emit, reads=(), writes=()):
        waits = self._waits(eng, self._deps(reads, writes, eng))
        self.count[eng] += 1
        tok = (eng, self.count[eng], eng)
        self.ops[eng].append((waits, emit, eng, 1))
        self._record(tok, reads, writes)
        return tok

    def dma(self, q, emit, reads=(), writes=(), persist=False):
        deps = self._deps(reads, writes)
        i = self.dma_rr[q]
        self.dma_rr[q] = (i + 1) % len(self.dma_sems[q])
        sk = (q, i)
        prev = self.dma_cnt[q][i]
        if prev > 0:
            deps.append((sk, 16 * prev, "dma"))
        waits = self._waits(q, deps)
        self.dma_cnt[q][i] = prev + 1
        tok = (sk, 16 * (prev + 1), "dma")
        self.ops[q].append((waits, emit, sk, 16))
        self._record(tok, reads, writes)
        if not persist:
            self.phase_dma.append(tok)
        return tok

    def end_phase(self):
        waits = self._waits("sync", self.phase_dma, skip_same_pe=False)
        self.ops["sync"].append((waits, None, None, 0))
        self.emit_all()
        self.ops = {e: [] for e in ENGS}
        self.phase_dma = []

    def final_wait(self, eng, bufs):
        deps = []
        for b in bufs:
            deps.extend((sk, v, e) for sk, (v, e) in b.w.items())
            deps.extend((sk, v, e) for sk, (v, e) in b.r.items())
        waits = self._waits(eng, deps, skip_same_pe=False)
        self.ops[eng].append((waits, None, None, 0))

    def emit_all(self):
        nc = self.nc
        with nc.Block() as block:
            def mk(ename):
                def body(eng):
                    for (waits, emit, sk, inc) in self.ops[ename]:
                        for (wsk, v) in waits:
                            eng.wait_ge(self._semobj(wsk), v)
                        if emit is not None:
                            inst = emit(eng)
                            inst.then_inc(self._semobj(sk), inc)
                return body
            block.sync(mk("sync"))
            block.tensor(mk("tensor"))
            block.vector(mk("vector"))
            block.scalar(mk("scalar"))
            block.gpsimd(mk("gpsimd"))


def build(stop=99, dbg=False):
    nc = bass.Bass("TRN2", target_bir_lowering=False)

    def din(name, shape, dt=F32):
        return nc.dram_tensor(name, list(shape), dt, kind="ExternalInput").ap()

    def dscr(name, shape, dt):
        return nc.dram_tensor(name, list(shape), dt, kind=("ExternalOutput" if dbg else "Internal")).ap()

    xf = din("xf", [T, D])
    xm = din("xm", [HALF, D])
    cvec = din("cvec", [128, 8, 2])
    wmod = din("wmod", [D, 6 * D])
    bmodT = din("bmodT", [128, 48])
    bmod = din("bmod", [1, 6 * D])
    ngT = din("ngT", [128, 8, 2])
    fng = din("fng", [1, D])
    win = din("win", [D, 1536])
    convw = din("convw", [128, 4, 4])
    convb = din("convb", [128, 4])
    lruw = din("lruw", [2, 2, 4, 128, 128])
    lrub = din("lrub", [128, 2, 2, 4])
    lrulam = din("lrulam", [128, 2, 4])
    gFR = din("gFR", [128, 8])
    wout = din("wout", [D, D])
    wq = din("wq", [D, 2048])
    keysT = din("keysT", [128, 16, 128])
    UL = din("UL", [NCH, 128, 1024])
    Vt = din("Vt", [NCH, 128, 1024])
    watab = din("watab", [128, 64, 2, 128])
    c64s = din("c64s", [64, 3, 32])
    bd64 = din("bd64", [128, 2, 128])
    sel = din("sel", [128, 2])
    out = nc.dram_tensor("out", [HALF, D], F32, kind="ExternalOutput").ap()

    Fd = dscr("Fd", [L, 512], BF16)
    Ud = dscr("Ud", [4, 128, T], F32)
    Gd = dscr("Gd", [4, 128, L], BF16)
    Bd = dscr("Bd", [2, 128, 64, 512], BF16)
    X1d = dscr("X1d", [HALF, D], F32)
    ULb = nc.dram_tensor("ULb", [NCH, 128, 1024], BF16, kind="Internal").ap()
    Vb = nc.dram_tensor("Vb", [NCH, 128, 1024], BF16, kind="Internal").ap()
    if dbg:
        dbgo = nc.dram_tensor("dbgo", [128, 16384], F32, kind="ExternalOutput").ap()

    es = ExitStack()
    with es:
        S = Sched(nc, es)
        dram_bufs = {}

        def dbuf(name):
            if name not in dram_bufs:
                dram_bufs[name] = Buf(None, name)
            return dram_bufs[name]

        def ACT(out_, in_, func, reads, writes, **kw):
            return S.op("scalar", lambda e: e.activation(out=out_, in_=in_, func=func, **kw), reads, writes)

        def VTS(out_, in0, s1, s2, op0, op1, reads, writes, eng="vector"):
            if s2 is None:
                return S.op(eng, lambda e: e.tensor_scalar(out=out_, in0=in0, scalar1=s1, scalar2=None, op0=op0), reads, writes)
            return S.op(eng, lambda e: e.tensor_scalar(out=out_, in0=in0, scalar1=s1, scalar2=s2, op0=op0, op1=op1), reads, writes)

        def VTT(out_, in0, in1, op, reads, writes, eng="vector"):
            return S.op(eng, lambda e: e.tensor_tensor(out=out_, in0=in0, in1=in1, op=op), reads, writes)

        def VSTT(out_, in0, sc, in1, op0, op1, reads, writes):
            return S.op("vector", lambda e: e.scalar_tensor_tensor(out=out_, in0=in0, scalar=sc, in1=in1, op0=op0, op1=op1), reads, writes)

        def VCOPY(out_, in_, reads, writes, eng="vector"):
            return S.op(eng, lambda e: e.tensor_copy(out=out_, in_=in_), reads, writes)

        def DMA(out_, in_, reads, writes, q="sync", persist=False):
            return S.dma(q, lambda e: e.dma_start(out=out_, in_=in_), reads, writes, persist=persist)

        def MM(outs, reads, writes):
            def emit(e):
                inst = None
                for (o, l, r, st, sp) in outs:
                    inst = e.matmul(o, lhsT=l, rhs=r, start=st, stop=sp)
                return inst
            return S.op("tensor", emit, reads, writes)

        def TR(outs, reads, writes):
            def emit(e):
                inst = None
                for (o, i, idn) in outs:
                    inst = e.transpose(out=o, in_=i, identity=idn)
                return inst
            return S.op("tensor", emit, reads, writes)

        PB = [S.psum("pb%d" % i, [128, 512], F32) for i in range(8)]

        ident_b = S.sbuf("ident_b", [128, 128], BF16)
        ident_f = S.sbuf("ident_f", [128, 128], F32)
        iota_f = S.sbuf("iota_f", [128, 128], F32)
        iota_b = S.sbuf("iota_b", [128, 128], BF16)
        ones_b = S.sbuf("ones_b", [128, 128], BF16)
        tmpc = S.sbuf("tmpc", [128, 128], F32)
        S.op("gpsimd", lambda e: e.iota(tmpc[:], pattern=[[1, 128]], base=0, channel_multiplier=-1,
                                        allow_small_or_imprecise_dtypes=True), [], [tmpc])
        VTS(ident_b[:], tmpc[:], 0.0, None, ALU.is_equal, None, [tmpc], [ident_b])
        VTS(ident_f[:], tmpc[:], 0.0, None, ALU.is_equal, None, [tmpc], [ident_f])
        S.op("gpsimd", lambda e: e.iota(iota_f[:], pattern=[[1, 128]], base=0, channel_multiplier=0,
                                        allow_small_or_imprecise_dtypes=True), [], [iota_f])
        VCOPY(iota_b[:], iota_f[:], [iota_f], [iota_b])
        S.op("vector", lambda e: e.memset(ones_b[:], 1.0), [], [ones_b])

        cv = S.sbuf("cv", [128, 8, 2], F32)
        scv = S.sbuf("scv", [128, 8, 2], F32)
        bmT = S.sbuf("bmT", [128, 48], F32)
        ng = S.sbuf("ng", [128, 8, 2], F32)
        modT = S.sbuf("modT", [128, 48, 2], F32)
        g2B = S.sbuf("g2B", [128, D], F32)
        fngB = S.sbuf("fngB", [128, D], F32)
        selt = S.sbuf("selt", [128, 2], F32)
        A1x = S.sbuf("A1x", [128, 8], F32)
        A1c = S.sbuf("A1c", [128, 8], F32)
        A2x = S.sbuf("A2x", [128, 8], F32)
        xt = [S.sbuf("xt%d" % i, [128, D], F32) for i in range(3)]
        xs = [S.sbuf("xs%d" % i, [128, D], BF16) for i in range(2)]
        junk = S.sbuf("junk", [128, D], BF16)
        ssq = [S.sbuf("ssq%d" % i, [128, 1], F32) for i in range(4)]
        rstd = [S.sbuf("rstd%d" % i, [128, 1], F32) for i in range(4)]
        cnt = {"xt": 0, "xs": 0, "ss": 0}
        g1_es = ExitStack()
        S.cur_es = g1_es
        g1B = S.sbuf("g1B", [128, D], F32)
        S.cur_es = es
        DMA(cv[:], cvec, [], [cv])
        DMA(bmT[:], bmodT, [], [bmT])
        DMA(ng[:], ngT, [], [ng])
        DMA(selt[:], sel, [], [selt])
        DMA(fngB[:], fng[0:1, :].partition_broadcast(128), [], [fngB])
        DMA(g1B[:], bmod[0:1, 2 * D:3 * D].partition_broadcast(128), [], [g1B])
        DMA(g2B[:], bmod[0:1, 5 * D:6 * D].partition_broadcast(128), [], [g2B])
        ACT(scv[:], cv[:], AF.Silu, [cv], [scv])

        def AP3(buf, rowlen, off, dims):
            return bass.AP(tensor=buf.t, offset=off, ap=[[rowlen, 128]] + [list(d) for d in dims])

        def phase_scope():
            ph = ExitStack()
            S.cur_es = ph
            return ph

        with phase_scope():
            wm = [S.sbuf("wm%d" % i, [128, 8, 512], F32) for i in range(2)]
            screp = S.sbuf("screp", [128, 8, 128], F32)
            for j in range(8):
                VCOPY(screp[:, j, :], scv[:, j, 0:1].to_broadcast([128, 128]), [scv], [screp])
            wmod_v = wmod.rearrange("(j p) c -> p j c", p=128)
            for grp in range(12):
                wmb = wm[grp % 2]
                DMA(wmb[:], wmod_v[:, :, grp * 512:(grp + 1) * 512], [], [wmb])
                pb = PB[grp % 2]
                outs = []
                for o4 in range(4):
                    for j in range(8):
                        outs.append((pb[:, o4 * 2:o4 * 2 + 2], wmb[:, j, o4 * 128:(o4 + 1) * 128], scv[:, j, :], j == 0, j == 7))
                MM(outs, [wmb, scv], [pb])
                oc0 = grp * 4
                for xc in range(2):
                    VTT(modT[:, oc0:oc0 + 4, xc], pb[:, xc:8:2], bmT[:, oc0:oc0 + 4], ALU.add, [pb, bmT], [modT])
                if grp in (4, 5, 10, 11):
                    pb2 = PB[2 + grp % 2]
                    outs = [(pb2[:, :], screp[:, j, :], wmb[:, j, :], j == 0, j == 7) for j in range(8)]
                    MM(outs, [wmb, screp], [pb2])
                    gB = g1B if grp < 6 else g2B
                    cs = (grp % 2) * 512
                    VTT(gB[:, cs:cs + 512], gB[:, cs:cs + 512], pb2[:, :], ALU.add, [pb2], [gB])
            VSTT(A1x[:], modT[:, 8:16, 0], 1.0, ng[:, :, 0], ALU.add, ALU.mult, [modT, ng], [A1x])
            VSTT(A1c[:], modT[:, 8:16, 1], 1.0, ng[:, :, 0], ALU.add, ALU.mult, [modT, ng], [A1c])
            VSTT(A2x[:], modT[:, 32:40, 0], 1.0, ng[:, :, 1], ALU.add, ALU.mult, [modT, ng], [A2x])
            S.end_phase()
        S.cur_es = es


        def rms_rstd(src_ap, src_buf, width):
            k = cnt["ss"] % 4
            cnt["ss"] += 1
            ACT(junk[:, 0:width], src_ap, AF.Square, [src_buf], [junk, ssq[k]], accum_out=ssq[k][:])
            ACT(rstd[k][:], ssq[k][:], AF.Sqrt, [ssq[k]], [rstd[k]], scale=1.0 / width, bias=EPS)
            S.op("vector", lambda e: e.reciprocal(out=rstd[k][:], in_=rstd[k][:]), [], [rstd[k]])
            return rstd[k]

        def norm_transpose(xb, banks, col0):
            r = rms_rstd(xb[:], xb, D)
            xsb = xs[cnt["xs"] % 2]
            cnt["xs"] += 1
            ACT(xsb[:], xb[:], AF.Copy, [xb, r], [xsb], scale=r[:])
            for bk in range(4):
                outs = []
                for jj in range(2):
                    j = bk * 2 + jj
                    o = banks[bk][:, :].bitcast(BF16)[:, jj * 512 + col0: jj * 512 + col0 + 128]
                    outs.append((o, xsb[:, j * 128:(j + 1) * 128], ident_b[:]))
                TR(outs, [xsb, ident_b], [banks[bk]])

        def load_x(src_rows):
            xb = xt[cnt["xt"] % 3]
            cnt["xt"] += 1
            DMA(xb[:], src_rows, [], [xb])
            return xb

        Fd_parts, Ud_parts, Gd_parts, UV_parts = [], [], [], []
        with phase_scope():
            winb = S.sbuf("winb", [128, 8, 1536], BF16)
            DMA(winb[:], win.rearrange("(j p) c -> p j c", p=128), [], [winb], q="gpsimd")
            hxT = [S.sbuf("hxT%d" % i, [128, 8, 512], BF16) for i in range(2)]
            fsb = [S.sbuf("fsb%d" % i, [128, 512], BF16) for i in range(2)]
            usb = [S.sbuf("usb%d" % i, [128, 512], F32) for i in range(2)]
            gsb = [S.sbuf("gsb%d" % i, [128, 512], BF16) for i in range(2)]
            k_f = k_u = k_g = k_p = 0
            xs1a = [S.sbuf("xs1a%d" % i, [128, D], BF16) for i in range(8)]

            def blk_geom(blk):
                if blk == 0:
                    return 0, 2, A1c, 1
                return CTX + (blk - 1) * 512, 4, A1x, 0

            def norm_part(blk):
                tok0, ntile, _, _ = blk_geom(blk)
                tiles = []
                for tt in range(ntile):
                    xb = load_x(xf[tok0 + tt * 128: tok0 + (tt + 1) * 128, :])
                    r = rms_rstd(xb[:], xb, D)
                    xsb = xs1a[(blk % 2) * 4 + tt]
                    ACT(xsb[:], xb[:], AF.Copy, [xb, r], [xsb], scale=r[:])
                    tiles.append(xsb)
                return tiles

            nxt_tiles = norm_part(0)
            for blk in range(17):
                tok0, ntile, Am, Sm_col = blk_geom(blk)
                ntok = ntile * 128
                hb = hxT[blk % 2]
                cur_tiles = nxt_tiles
                for tt in range(ntile):
                    xsb = cur_tiles[tt]
                    for bk in range(4):
                        outs = []
                        for jj in range(2):
                            j = bk * 2 + jj
                            o = PB[bk][:, :].bitcast(BF16)[:, jj * 512 + tt * 128: jj * 512 + tt * 128 + 128]
                            outs.append((o, xsb[:, j * 128:(j + 1) * 128], ident_b[:]))
                        TR(outs, [xsb, ident_b], [PB[bk]])
                if blk + 1 < 17:
                    nxt_tiles = norm_part(blk + 1)
                for j in range(8):
                    src = PB[j // 2][:, :].bitcast(BF16)[:, (j % 2) * 512:(j % 2) * 512 + ntok]
                    VTS(hb[:, j, 0:ntok], src, Am[:, j:j + 1], modT[:, j, Sm_col:Sm_col + 1], ALU.mult, ALU.add,
                        [PB[j // 2], Am, modT], [hb])
                if blk > 0:
                    lat0 = tok0 - CTX
                    for tt in range(ntile):
                        pb = PB[4 + k_p % 2]
                        k_p += 1
                        MM([(pb[:, :], hb[:, j, tt * 128:(tt + 1) * 128], winb[:, j, 0:512], j == 0, j == 7) for j in range(8)],
                           [hb, winb], [pb])
                        fb = fsb[k_f % 2]
                        k_f += 1
                        VCOPY(fb[:], pb[:, :], [pb], [fb])
                        part = Buf(None)
                        Fd_parts.append(part)
                        DMA(Fd[lat0 + tt * 128: lat0 + (tt + 1) * 128, :], fb[:], [fb], [part], q="gpsimd")
                for ct in range(8):
                    if ct >= 4 and blk == 0:
                        continue
                    pb = PB[6 + k_p % 2]
                    k_p += 1
                    MM([(pb[:, 0:ntok], winb[:, j, 512 + ct * 128: 512 + (ct + 1) * 128], hb[:, j, 0:ntok], j == 0, j == 7) for j in range(8)],
                       [hb, winb], [pb])
                    if ct < 4:
                        ub = usb[k_u % 2]
                        k_u += 1
                        VCOPY(ub[:, 0:ntok], pb[:, 0:ntok], [pb], [ub])
                        part = Buf(None)
                        Ud_parts.append(part)
                        DMA(Ud[ct, :, tok0:tok0 + ntok], ub[:, 0:ntok], [ub], [part], q="gpsimd")
                    else:
                        gb = gsb[k_g % 2]
                        k_g += 1
                        ACT(gb[:, 0:ntok], pb[:, 0:ntok], AF.Gelu_apprx_tanh, [pb], [gb])
                        part = Buf(None)
                        Gd_parts.append(part)
                        DMA(Gd[ct - 4, :, tok0 - CTX: tok0 - CTX + ntok], gb[:, 0:ntok], [gb], [part], q="gpsimd")
            S.end_phase()
        S.cur_es = es
        if stop <= 1:
            g1_es.close()
            return nc

        mix_es = ExitStack()
        S.cur_es = mix_es
        RX = [S.sbuf("RX%d" % i, [128, HALF], BF16) for i in range(4)]

        with phase_scope():
            CHW = 1024
            NLC = L // CHW
            HF = S.sbuf("HF", [128, L], F32)
            HFb = [HF, HF]
            wk = {}
            for nm in ("UU", "UC", "R", "Q", "H", "HB"):
                wk[(0, nm)] = [S.sbuf("%s_%d" % (nm, i), [128, CHW], F32) for i in range(2)]
                wk[(1, nm)] = wk[(0, nm)]
            GGb = [S.sbuf("GGb%d" % i, [128, CHW], BF16) for i in range(2)]
            lw = S.sbuf("lw", [128, 16, 128], F32)
            cw = S.sbuf("cw", [128, 4, 4], F32)
            cb = S.sbuf("cb", [128, 4], F32)
            lb = S.sbuf("lb", [128, 2, 2, 4], F32)
            lam = S.sbuf("lam", [128, 2, 4], F32)
            sp8 = S.sbuf("sp8", [128, 2, 4], F32)
            sp16 = S.sbuf("sp16", [128, 2, 4], F32)
            DMA(lw[:], lruw.rearrange("d g c k m -> k (d g c) m"), [], [lw])
            conv_jobs = []
            if stop > 4:
                for c8 in range(32):
                    for src_, dst_ in ((UL, ULb), (Vt, Vb)):
                        conv_jobs.append((dst_[c8 * 4:(c8 + 1) * 4], src_[c8 * 4:(c8 + 1) * 4]))
            conv_jobs.reverse()

            def conv_step(gate):
                if conv_jobs:
                    dst_, src_ = conv_jobs.pop()
                    part = Buf(None)
                    UV_parts.append(part)
                    DMA(dst_, src_, [gate], [part], q="gpsimd", persist=True)
            DMA(cw[:], convw, [], [cw])
            DMA(cb[:], convb, [], [cb])
            DMA(lb[:], lrub, [], [lb])
            DMA(lam[:], lrulam, [], [lam])
            ACT(sp8[:], lam[:], AF.Exp, [lam], [sp8], scale=-1.0)
            ACT(sp8[:], sp8[:], AF.Ln, [], [sp8], bias=1.0)
            VTS(sp16[:], sp8[:], -16.0, None, ALU.mult, None, [sp8], [sp16])
            VTS(sp8[:], sp8[:], -8.0, None, ALU.mult, None, [], [sp8])
            chunks = [(0, CTX)] + [(CTX + k * CHW, CHW) for k in range(NLC)]
            kk = {"p": 0, "w0": 0, "w1": 0, "g": 0}

            def lru_pass(ct, d):
                HFc = HFb[ct % 2]
                order = list(range(NLC + 1)) if d == 0 else [0] + list(range(NLC, 0, -1))
                st = {"carry": None}
                held = {}

                def stage_a(ci):
                    c0, cl = chunks[ci]
                    w = kk["w0"] % 2
                    kk["w0"] += 1
                    UU, UC, R, Q, H = (wk[(d, nm)][w] for nm in ("UU", "UC", "R", "Q", "H"))
                    HB = wk[(1, "HB")][w] if d == 1 else None
                    held[ci] = (c0, cl, UU, UC, R, Q, H, HB)
                    DMA(UU[:, 0:cl], Ud[ct, :, c0:c0 + cl], Ud_parts, [UU])
                    gate = Buf(None)
                    ACT(UC[:, 0:cl], UU[:, 0:cl], AF.Identity, [UU, cw, cb], [UC, gate], scale=cw[:, ct, 2:3], bias=cb[:, ct:ct + 1])
                    conv_step(gate)
                    for k, o in ((0, -2), (1, -1), (3, 1)):
                        a = abs(o)
                        if ci == 0:
                            if o < 0:
                                oap, iap = UC[:, a:cl], UU[:, 0:cl - a]
                            else:
                                oap, iap = UC[:, 0:cl - a], UU[:, a:cl]
                        else:
                            nr = cl // 64
                            if o < 0:
                                oap = AP3(UC, CHW, a, [[64, nr], [1, 64 - a]])
                                iap = AP3(UU, CHW, 0, [[64, nr], [1, 64 - a]])
                            else:
                                oap = AP3(UC, CHW, 0, [[64, nr], [1, 64 - a]])
                                iap = AP3(UU, CHW, a, [[64, nr], [1, 64 - a]])
                        VSTT(oap, iap, cw[:, ct, k:k + 1], oap, ALU.mult, ALU.add, [UU, cw], [UC])
                    sw = min(512, cl)
                    for sub in range(cl // sw):
                        for g, dst in ((0, R), (1, H)):
                            pb = PB[kk["p"] % 8]
                            kk["p"] += 1
                            MM([(pb[:, 0:sw], lw[:, (d * 2 + g) * 4 + ct, :], UC[:, sub * sw:(sub + 1) * sw], True, True)], [lw, UC], [pb])
                            ACT(dst[:, sub * sw:(sub + 1) * sw], pb[:, 0:sw], AF.Sigmoid, [pb, lb], [dst], bias=lb[:, d, g, ct:ct + 1])

                def stage_b(ci):
                    c0, cl, UU, UC, R, Q, H, HB = held.pop(ci)
                    carry = st["carry"]
                    ACT(Q[:, 0:cl], R[:, 0:cl], AF.Exp, [R, sp16], [Q], scale=sp16[:, d, ct:ct + 1])
                    ACT(R[:, 0:cl], R[:, 0:cl], AF.Exp, [sp8], [R], scale=sp8[:, d, ct:ct + 1])
                    ACT(Q[:, 0:cl], Q[:, 0:cl], AF.Sqrt, [], [Q], scale=-1.0, bias=1.0)
                    VTT(H[:, 0:cl], H[:, 0:cl], UC[:, 0:cl], ALU.mult, [UC], [H])
                    VTT(H[:, 0:cl], H[:, 0:cl], Q[:, 0:cl], ALU.mult, [Q], [H])
                    init = 0.0 if carry is None else carry[1]
                    rds = [R, H] + ([] if carry is None else [carry[0]])
                    if d == 0:
                        if ci == 0:
                            ob, oap, cap = Q, Q[:, 0:cl], Q[:, cl - 1:cl]
                        else:
                            l0 = c0 - CTX
                            ob, oap, cap = HFc, HFc[:, l0:l0 + cl], HFc[:, l0 + cl - 1:l0 + cl]
                        S.op("vector", lambda e, oap=oap, R=R, H=H, init=init, cl=cl: e.tensor_tensor_scan(
                            out=oap, data0=R[:, 0:cl], data1=H[:, 0:cl], initial=init, op0=ALU.mult, op1=ALU.add), rds, [ob])
                        st["carry"] = (ob, cap)
                    else:
                        ob = Q if ci == 0 else HB
                        S.op("vector", lambda e, ob=ob, R=R, H=H, init=init, cl=cl: e.tensor_tensor_scan(
                            out=ob[:, cl - 1::-1], data0=R[:, cl - 1::-1], data1=H[:, cl - 1::-1], initial=init,
                            op0=ALU.mult, op1=ALU.add), rds, [ob])
                        st["carry"] = (ob, ob[:, 0:1])
                        if ci > 0:
                            l0 = c0 - CTX
                            GG = GGb[kk["g"] % 2]
                            kk["g"] += 1
                            DMA(GG[:], Gd[ct, :, l0:l0 + cl], Gd_parts, [GG])
                            VTT(H[:], HB[:], HFc[:, l0:l0 + cl], ALU.add, [HB, HFc], [H])
                            VTT(H[:], H[:], GG[:], ALU.mult, [GG], [H])
                            half, pos0 = (ci - 1) // (NLC // 2), ((ci - 1) % (NLC // 2)) * CHW
                            if half == 1:
                                VTS(RX[ct][:, pos0:pos0 + cl], H[:], selt[:, 1:2], None, ALU.mult, None, [H, selt], [RX[ct]])
                            else:
                                VSTT(RX[ct][:, pos0:pos0 + cl], H[:], selt[:, 0:1], RX[ct][:, pos0:pos0 + cl],
                                     ALU.mult, ALU.add, [H, selt], [RX[ct]])

                stage_a(order[0])
                for idx, ci in enumerate(order):
                    if idx + 1 < len(order):
                        stage_a(order[idx + 1])
                    stage_b(ci)
                    yield

            for ct in range(4):
                for d in range(2):
                    for _ in lru_pass(ct, d):
                        pass
            while conv_jobs:
                conv_step(Buf(None))
            if dbg:
                for ct in range(4):
                    VCOPY(HF[:, (ct % 2) * 4096:(ct % 2) * 4096 + 4096], RX[ct][:], [RX[ct]], [HF])
                    DMA(dbgo[:, ct * 4096:(ct + 1) * 4096], HF[:, (ct % 2) * 4096:(ct % 2) * 4096 + 4096], [HF], [Buf(None)])
            S.end_phase()
        S.cur_es = es
        if stop <= 2:
            mix_es.close()
            g1_es.close()
            return nc

        S.cur_es = mix_es
        YT = [S.sbuf("YT%d" % i, [128, HALF], BF16) for i in range(4)]
        Bd_parts = []
        with phase_scope():
            WA = S.sbuf("WA", [128, 64 * 2 * 128], BF16)
            for q4 in range(4):
                DMA(WA[:, q4 * 4096:(q4 + 1) * 4096], watab[:, q4 * 16:(q4 + 1) * 16, :, :].rearrange("p a b c -> p (a b c)"),
                    [], [WA], q="gpsimd")
            fa = [S.sbuf("fa%d" % i, [128, 512], BF16) for i in range(3)]
            bsb = [S.sbuf("bsb%d" % i, [128, 2, 512], BF16) for i in range(2)]
            Fd_v = Fd.rearrange("(a b) c -> a b c", b=64)
            for n2 in range(64):
                f = fa[n2 % 3]
                DMA(f[:], Fd_v[:, n2, :], Fd_parts, [f])
                pr, pi = PB[(n2 % 2) * 2], PB[(n2 % 2) * 2 + 1]
                MM([(pr[:, :], WA[:, (n2 * 2 + 0) * 128:(n2 * 2 + 1) * 128], f[:], True, True)], [WA, f], [pr])
                MM([(pi[:, :], WA[:, (n2 * 2 + 1) * 128:(n2 * 2 + 2) * 128], f[:], True, True)], [WA, f], [pi])
                bs = bsb[n2 % 2]
                ACT(bs[:, 0, :], pr[:, :], AF.Copy, [pr], [bs])
                VCOPY(bs[:, 1, :], pi[:, :], [pi], [bs])
                part = Buf(None)
                Bd_parts.append(part)
                DMA(Bd[:, :, n2, :].rearrange("r k c -> k r c"), bs[:], [bs], [part], q="gpsimd")
            S.end_phase()
        S.cur_es = es
        with phase_scope():
            C64 = S.sbuf("C64", [64, 3, 32], BF16)
            BD = S.sbuf("BD", [128, 2, 128], BF16)
            DMA(C64[:], c64s, [], [C64], q="gpsimd")
            DMA(BD[:], bd64, [], [BD], q="gpsimd")
            XT = [[[S.sbuf("XT%d_%d_%d" % (w_, ri, cc), [128, 512], BF16) for cc in range(4)] for ri in range(2)] for w_ in range(2)]
            bt = [S.sbuf("bt%d" % i, [64, 2, 512], BF16) for i in range(3)]
            for k1 in range(128):
                b_ = bt[k1 % 3]
                DMA(b_[:], Bd[:, k1, :, :].rearrange("r n c -> n r c"), Bd_parts, [b_])
                g, pos = k1 // 16, k1 % 16
                for cc in range(4):
                    cs = slice(cc * 128, (cc + 1) * 128)
                    MM([(PB[cc][:, pos * 32:(pos + 1) * 32], b_[:, 0, cs], C64[:, 0, :], True, False),
                        (PB[cc][:, pos * 32:(pos + 1) * 32], b_[:, 1, cs], C64[:, 1, :], False, True)], [b_, C64], [PB[cc]])
                    MM([(PB[4 + cc][:, pos * 32:(pos + 1) * 32], b_[:, 1, cs], C64[:, 0, :], True, False),
                        (PB[4 + cc][:, pos * 32:(pos + 1) * 32], b_[:, 0, cs], C64[:, 2, :], False, True)], [b_, C64], [PB[4 + cc]])
                if pos == 15:
                    X = XT[g % 2]
                    ts_ = slice(g * 512, (g + 1) * 512)
                    for cc in range(4):
                        ACT(X[0][cc][:], PB[cc][:, :], AF.Copy, [PB[cc]], [X[0][cc]])
                        VCOPY(X[1][cc][:], PB[4 + cc][:, :], [PB[4 + cc]], [X[1][cc]])
                    for cc in range(4):
                        pb = PB[cc]
                        MM([(pb[:, :], BD[:, 0, :], X[0][cc][:], True, False),
                            (pb[:, :], BD[:, 1, :], X[1][cc][:], False, True)], [BD, X[0][cc], X[1][cc]], [pb])
                        if cc % 2 == 0:
                            ACT(YT[cc][:, ts_], pb[:, :], AF.Copy, [pb], [YT[cc]])
                        else:
                            VCOPY(YT[cc][:, ts_], pb[:, :], [pb], [YT[cc]])
            if dbg and stop == 3:
                for cc in range(4):
                    dtmp = S.sbuf("dtmp%d" % cc, [128, HALF], F32)
                    VCOPY(dtmp[:], YT[cc][:], [YT[cc]], [dtmp])
                    DMA(dbgo[:, cc * 4096:(cc + 1) * 4096], dtmp[:], [dtmp], [Buf(None)])
            S.end_phase()
        S.cur_es = es
        if stop <= 3:
            mix_es.close()
            g1_es.close()
            return nc

        X1_parts = []
        with phase_scope():
            gfr = S.sbuf("gfr", [128, 8], F32)
            DMA(gfr[:], gFR, [], [gfr])
            woutb = S.sbuf("woutb", [128, 8, D], BF16)
            DMA(woutb[:], wout.rearrange("(j p) c -> p j c", p=128), [], [woutb], q="gpsimd")
            for j in range(8):
                VTT(woutb[:, j, :], woutb[:, j, :], g1B[:], ALU.mult, [g1B], [woutb])
            sq = [S.sbuf("sq%d" % i, [128, 512], BF16) for i in range(2)]
            rs = [S.sbuf("rs%d" % i, [128, 512], F32) for i in range(2)]
            kq = 0
            for gi, grp in enumerate((YT, RX)):
                for tb in range(8):
                    ts_ = slice(tb * 512, (tb + 1) * 512)
                    pb = PB[tb % 2]
                    for cc in range(4):
                        q_ = sq[kq % 2]
                        kq += 1
                        ACT(q_[:], grp[cc][:, ts_], AF.Square, [grp[cc]], [q_])
                        MM([(pb[:, :], ones_b[:], q_[:], cc == 0, cc == 3)], [ones_b, q_], [pb])
                    r_ = rs[tb % 2]
                    ACT(r_[:], pb[:, :], AF.Sqrt, [pb], [r_], scale=1.0 / 512, bias=EPS)
                    S.op("vector", lambda e, r_=r_: e.reciprocal(out=r_[:], in_=r_[:]), [], [r_])
                    for cc in range(4):
                        VSTT(grp[cc][:, ts_], grp[cc][:, ts_], gfr[:, gi * 4 + cc: gi * 4 + cc + 1], r_[:], ALU.mult, ALU.mult,
                             [r_, gfr], [grp[cc]])
            x1sb = [S.sbuf("x1sb%d" % i, [128, D], F32) for i in range(2)]
            for tt in range(32):
                xb = load_x(xm[tt * 128:(tt + 1) * 128, :])
                x1 = x1sb[tt % 2]
                for dh in range(2):
                    pb = PB[2 + (tt % 2) * 2 + dh]
                    outs = []
                    for cc in range(4):
                        outs.append((pb[:, :], YT[cc][:, tt:HALF:32], woutb[:, cc, dh * 512:(dh + 1) * 512], cc == 0, False))
                    for cc in range(4):
                        outs.append((pb[:, :], RX[cc][:, tt * 128:(tt + 1) * 128], woutb[:, 4 + cc, dh * 512:(dh + 1) * 512], False, cc == 3))
                    MM(outs, YT + RX + [woutb], [pb])
                    VTT(x1[:, dh * 512:(dh + 1) * 512], pb[:, :], xb[:, dh * 512:(dh + 1) * 512], ALU.add, [pb, xb], [x1])
                part = Buf(None)
                X1_parts.append(part)
                DMA(X1d[tt * 128:(tt + 1) * 128, :], x1[:], [x1], [part], q="gpsimd")
            S.end_phase()
        S.cur_es = es
        mix_es.close()
        g1_es.close()
        if stop <= 4:
            return nc

        out_parts = []
        with phase_scope():
            wqb = S.sbuf("wqb", [128, 8, 2048], BF16)
            DMA(wqb[:], wq.rearrange("(j p) c -> p j c", p=128), [], [wqb], q="gpsimd")
            kT = S.sbuf("kT", [128, 16, 128], BF16)
            DMA(kT[:], keysT, [], [kT], q="gpsimd")
            G_all = S.sbuf("G_all", [128, TB, 128], BF16)
            h2Tb = [S.sbuf("h2T%d" % i, [128, 8, TB], BF16) for i in range(2)]
            qT = S.sbuf("qT", [128, 16, TB], BF16)
            S_sb = S.sbuf("S_sb", [128, 2048], F32)
            S2big = S.sbuf("S2big", [128, 2048], F32)
            S2s = [S.alias(S2big) for i in range(16)]
            c2s = [S.alias(S2big) for i in range(8)]

            def S2ap(hp):
                return S2big[:, hp * 128:(hp + 1) * 128]

            def c2ap(h):
                return S2big[:, h * 256:(h + 1) * 256]
            Vv = S.sbuf("Vv", [128, 8, 2, 16], F32)
            Iu = S.sbuf("Iu", [128, 8, 2, 16], U32)
            If_ = S.sbuf("If", [128, 8, 2, 16], F32)
            cand = S.sbuf("cand", [128, 8, 256], F32)
            SC = S.sbuf("SC", [128, 8, 16], F32)
            Pu = S.sbuf("Pu", [128, 8, 16], U32)
            PA = S.sbuf("PA", [128, 8, 16], U32)
            PBu = S.sbuf("PBu", [128, 8, 16], U32)
            PAf = S.sbuf("PAf", [128, 128], F32)
            PBf = S.sbuf("PBf", [128, 128], F32)
            Ee = S.sbuf("Ee", [128, 8, 16], F32)
            Vv_a = [S.alias(Vv) for _ in range(16)]
            Vv_b = [S.alias(Vv) for _ in range(16)]
            Iu_a = [S.alias(Iu) for _ in range(16)]
            Iu_b = [S.alias(Iu) for _ in range(16)]
            SC_a = [S.alias(SC) for _ in range(8)]
            SC_b = [S.alias(SC) for _ in range(8)]
            Pu_a = [S.alias(Pu) for _ in range(8)]
            Pu_b = [S.alias(Pu) for _ in range(8)]
            Zs = S.sbuf("Zs", [128, 8], F32)
            I12G = [S.sbuf("I12G%d" % i, [128, 128], F32) for i in range(3)]
            IGT = [S.sbuf("IGT%d" % i, [128, TB], F32) for i in range(3)]
            NOH = 4
            lt = [S.sbuf("lt%d" % i, [128, 128], BF16) for i in range(NOH)]
            rt = [S.sbuf("rt%d" % i, [128, 128], BF16) for i in range(NOH)]
            NUT, NVC, NGA, NWT = 4, 4, 2, 3
            UTb = [S.sbuf("UTb%d" % i, [128, 8, 128], BF16) for i in range(NUT)]
            Vcb = [S.sbuf("Vcb%d" % i, [128, D], BF16) for i in range(NVC)]
            gab = [S.sbuf("gab%d" % i, [128, TB], BF16) for i in range(NGA)]
            Wtb = [S.sbuf("Wtb%d" % i, [128, TB], BF16) for i in range(NWT)]
            NTT = TB // 128
            NBLK = HALF // TB
            if dbg and stop == 5:
                NBLK = 2
            PQ = PB[7]

            def gen_topk(blk):
                h2T = h2Tb[blk % 2]
                for tt in range(NTT):
                    r0 = blk * TB + tt * 128
                    xb = load_x(X1d[r0:r0 + 128, :])
                    r = rms_rstd(xb[:], xb, D)
                    yield
                    xsb = xs[cnt["xs"] % 2]
                    cnt["xs"] += 1
                    ACT(xsb[:], xb[:], AF.Copy, [xb, r], [xsb], scale=r[:])
                    yield
                    yield
                    pq16 = PQ[:, :].bitcast(BF16)
                    TR([(pq16[:, j * 128:(j + 1) * 128], xsb[:, j * 128:(j + 1) * 128], ident_b[:]) for j in range(8)], [xsb, ident_b], [PQ])
                    yield
                    for j in range(8):
                        VTS(h2T[:, j, tt * 128:(tt + 1) * 128], pq16[:, j * 128:(j + 1) * 128], A2x[:, j:j + 1], modT[:, 24 + j, 0:1],
                            ALU.mult, ALU.add, [PQ, A2x, modT], [h2T])
                    yield
                    yield
                for hp2 in range(8):
                    outs = []
                    for q in range(2):
                        hp = hp2 * 2 + q
                        outs += [(PQ[:, q * TB:(q + 1) * TB], wqb[:, j, hp * 128:(hp + 1) * 128], h2T[:, j, :], j == 0, j == 7) for j in range(8)]
                    MM(outs, [wqb, h2T], [PQ])
                    yield
                    VCOPY(qT[:, hp2 * 2:hp2 * 2 + 2, :], PQ[:, 0:2 * TB].rearrange("p (q t) -> p q t", q=2), [PQ], [qT])
                    yield
                for tt in range(NTT):
                    tsl = slice(tt * 128, (tt + 1) * 128)
                    for b4 in range(4):
                        MM([(PQ[:, q * 128:(q + 1) * 128], qT[:, b4 * 4 + q, tsl], kT[:, b4 * 4 + q, :], True, True) for q in range(4)],
                           [qT, kT], [PQ])
                        yield
                        VCOPY(S_sb[:, b4 * 512:(b4 + 1) * 512], PQ[:, :], [PQ], [S_sb])
                        yield
                    hps = [(hp, hp // 2, hp % 2, S_sb[:, hp * 128:(hp + 1) * 128]) for hp in range(16)]
                    for hp, h_, p_, srow in hps:
                        S.op("vector", lambda e, h_=h_, p_=p_, srow=srow: e.max(out=Vv[:, h_, p_, 0:8], in_=srow), [S_sb], [Vv_a[hp]])
                    yield
                    for hp, h_, p_, srow in hps:
                        S.op("vector", lambda e, h_=h_, p_=p_, srow=srow: e.max_index(out=Iu[:, h_, p_, 0:8], in_max=Vv[:, h_, p_, 0:8], in_values=srow), [S_sb, Vv_a[hp]], [Iu_a[hp]])
                    yield
                    for hp, h_, p_, srow in hps:
                        S.op("vector", lambda e, hp=hp, h_=h_, p_=p_, srow=srow: e.match_replace(out=S2ap(hp), in_to_replace=Vv[:, h_, p_, 0:8], in_values=srow, imm_value=-1e30), [S_sb, Vv_a[hp]], [S2s[hp], c2s[hp // 2]])
                    yield
                    for hp, h_, p_, srow in hps:
                        S.op("vector", lambda e, hp=hp, h_=h_, p_=p_: e.max(out=Vv[:, h_, p_, 8:16], in_=S2ap(hp)), [S2s[hp]], [Vv_b[hp]])
                    yield
                    for hp, h_, p_, srow in hps:
                        S.op("vector", lambda e, hp=hp, h_=h_, p_=p_: e.max_index(out=Iu[:, h_, p_, 8:16], in_max=Vv[:, h_, p_, 8:16], in_values=S2ap(hp)), [S2s[hp], Vv_b[hp]], [Iu_b[hp]])
                    yield
                    VCOPY(If_[:], Iu[:], Iu_a + Iu_b, [If_])
                    cand4 = cand[:, :, :].rearrange("p h (a b) -> p h a b", b=16)
                    VTT(cand4, Vv[:, :, 0, :].unsqueeze(3).to_broadcast([128, 8, 16, 16]),
                        Vv[:, :, 1, :].unsqueeze(2).to_broadcast([128, 8, 16, 16]), ALU.add, Vv_a + Vv_b, [cand])
                    yield
                    for h_ in range(8):
                        S.op("vector", lambda e, h_=h_: e.max(out=SC[:, h_, 0:8], in_=cand[:, h_, :]), [cand], [SC_a[h_]])
                    yield
                    for h_ in range(8):
                        S.op("vector", lambda e, h_=h_: e.max_index(out=Pu[:, h_, 0:8], in_max=SC[:, h_, 0:8], in_values=cand[:, h_, :]), [cand, SC_a[h_]], [Pu_a[h_]])
                    for h_ in range(8):
                        S.op("vector", lambda e, h_=h_: e.match_replace(out=c2ap(h_), in_to_replace=SC[:, h_, 0:8], in_values=cand[:, h_, :], imm_value=-1e30), [cand, SC_a[h_]], [c2s[h_], S2s[2 * h_], S2s[2 * h_ + 1]])
                    yield
                    for h_ in range(8):
                        S.op("vector", lambda e, h_=h_: e.max(out=SC[:, h_, 8:16], in_=c2ap(h_)), [c2s[h_]], [SC_b[h_]])
                    yield
                    for h_ in range(8):
                        S.op("vector", lambda e, h_=h_: e.max_index(out=Pu[:, h_, 8:16], in_max=SC[:, h_, 8:16], in_values=c2ap(h_)), [c2s[h_], SC_b[h_]], [Pu_b[h_]])
                    VTT(Ee[:], SC[:], SC[:, :, 0:1].to_broadcast([128, 8, 16]), ALU.subtract, SC_a + SC_b, [Ee])
                    yield
                    ACT(Ee[:], Ee[:], AF.Exp, [], [Ee])
                    VTS(PA[:], Pu[:], 4, None, ALU.logical_shift_right, None, Pu_a + Pu_b, [PA])
                    VTS(PBu[:], Pu[:], 15, None, ALU.bitwise_and, None, Pu_a + Pu_b, [PBu])
                    yield
                    VCOPY(PAf[:, :].rearrange("p (h k) -> p h k", k=16), PA[:], [PA], [PAf])
                    VCOPY(PBf[:, :].rearrange("p (h k) -> p h k", k=16), PBu[:], [PBu], [PBf])
                    S.op("vector", lambda e: e.tensor_reduce(out=Zs[:], in_=Ee[:], axis=AX.X, op=ALU.add), [Ee], [Zs])
                    yield
                    S.op("vector", lambda e: e.reciprocal(out=Zs[:], in_=Zs[:]), [], [Zs])
                    yield
                    GW = I12G[2]
                    VTT(GW[:, :].rearrange("p (h k) -> p h k", k=16), Ee[:], Zs[:, :].unsqueeze(2).to_broadcast([128, 8, 16]), ALU.mult,
                        [Ee, Zs], [GW])
                    for which, (Pf, p_) in enumerate(((PAf, 0), (PBf, 1))):
                        EQ3 = cand[:, :, :].rearrange("p h (k a) -> p (h k) a", a=16)
                        VTT(EQ3, iota_f[:, 0:16].unsqueeze(1).to_broadcast([128, 128, 16]),
                            Pf[:, :].unsqueeze(2).to_broadcast([128, 128, 16]), ALU.is_equal, [iota_f, Pf], [cand])
                        yield
                        EQ4 = cand[:, :, :].rearrange("p h (k a) -> p h k a", a=16)
                        VTT(EQ4, EQ4, If_[:, :, p_, :].unsqueeze(2).to_broadcast([128, 8, 16, 16]), ALU.mult, [If_], [cand])
                        yield
                        S.op("vector", lambda e, which=which, EQ3=EQ3: e.tensor_reduce(out=I12G[which][:], in_=EQ3, axis=AX.X, op=ALU.add),
                             [cand], [I12G[which]])
                        yield
                    if dbg and blk == 0 and tt == 0:
                        for q in range(3):
                            DMA(dbgo[:, q * 128:(q + 1) * 128], I12G[q][:], [I12G[q]], [Buf(None)])
                        DMA(dbgo[:, 384:384 + 128], SC[:, :, :].rearrange("p h k -> p (h k)"), SC_a + SC_b, [Buf(None)])
                        DMA(dbgo[:, 2048:4096], S_sb[:], [S_sb], [Buf(None)])
                    yield
                    MM([(PQ[:, q * 128:(q + 1) * 128], I12G[q][:], ident_f[:], True, True) for q in range(3)], I12G + [ident_f], [PQ])
                    yield
                    for q in range(3):
                        VCOPY(IGT[q][:, tsl], PQ[:, q * 128:(q + 1) * 128], [PQ], [IGT[q]])
                    yield

            def gbuild(blk):
                for t in range(TB):
                    l_, r_ = lt[t % NOH], rt[t % NOH]
                    if t % 3 == 0:
                        VTS(l_[:], iota_b[:], IGT[0][:, t:t + 1], None, ALU.is_equal, None, [iota_b, IGT[0]], [l_])
                        ACT(l_[:], l_[:], AF.Copy, [IGT[2]], [l_], scale=IGT[2][:, t:t + 1])
                    else:
                        VSTT(l_[:], iota_b[:], IGT[0][:, t:t + 1], IGT[2][:, t:t + 1].to_broadcast([128, 128]), ALU.is_equal, ALU.mult,
                             [iota_b, IGT[0], IGT[2]], [l_])
                    VTS(r_[:], iota_b[:], IGT[1][:, t:t + 1], None, ALU.is_equal, None, [iota_b, IGT[1]], [r_])
                    pg = PB[4 + (t // 4) % 3]
                    MM([(pg[:, (t % 4) * 128:(t % 4 + 1) * 128], r_[:], l_[:], True, True)], [l_, r_], [pg])
                    if t % 4 == 3:
                        t0 = t - 3
                        ACT(G_all[:, t0:t0 + 4, :], pg[:, :].rearrange("p (t i) -> p t i", i=128), AF.Copy, [pg], [G_all])

            def sweep(blk, filler):
                h2T = h2Tb[blk % 2]
                LOOK = 2
                NAB = 3

                def a_mm(i):
                    ut = UTb[i % NUT]
                    DMA(ut[:], ULb[i].rearrange("p (j e) -> p j e", e=128), UV_parts, [ut])
                    pa = PB[4 + i % NAB]
                    MM([(pa[:, 0:TB], ut[:, j, :], h2T[:, j, :], j == 0, j == 7) for j in range(8)], [ut, h2T], [pa])
                for i in range(LOOK):
                    a_mm(i)
                DMA(Vcb[0][:], Vb[0], UV_parts, [Vcb[0]])
                for i in range(NCH):
                    vc = Vcb[i % NVC]
                    if i + 1 < NCH:
                        DMA(Vcb[(i + 1) % NVC][:], Vb[i + 1], UV_parts, [Vcb[(i + 1) % NVC]])
                    if i + LOOK < NCH:
                        a_mm(i + LOOK)
                    pa = PB[4 + i % NAB]
                    ga = gab[i % NGA]
                    ACT(ga[:], pa[:, 0:TB], AF.Gelu_apprx_tanh, [pa], [ga])
                    wt = Wtb[i % NWT]
                    VTT(wt[:], ga[:], G_all[:, :, i], ALU.mult, [ga, G_all], [wt])
                    outs = []
                    for tt in range(NTT):
                        for dh in range(2):
                            outs.append((PB[tt * 2 + dh][:, :], wt[:, tt * 128:(tt + 1) * 128], vc[:, dh * 512:(dh + 1) * 512], i == 0, i == NCH - 1))
                    MM(outs, [wt, vc], PB[0:2 * NTT])
                    if filler is not None and i >= 2:
                        next(filler, None)

            def epilogue(blk):
                for tt in range(NTT):
                    r0 = blk * TB + tt * 128
                    xb = load_x(X1d[r0:r0 + 128, :])
                    for dh in range(2):
                        pb = PB[tt * 2 + dh]
                        cs = slice(dh * 512, (dh + 1) * 512)
                        VTT(pb[:, :], pb[:, :], g2B[:, cs], ALU.mult, [g2B], [pb])
                        VTT(xb[:, cs], xb[:, cs], pb[:, :], ALU.add, [pb], [xb])
                    r = rms_rstd(xb[:], xb, D)
                    VSTT(xb[:], xb[:], r[:], fngB[:], ALU.mult, ALU.mult, [r, fngB], [xb])
                    part = Buf(None)
                    out_parts.append(part)
                    DMA(out[r0:r0 + 128, :], xb[:], [xb], [part], q="gpsimd")

            for _ in gen_topk(0):
                pass
            for blk in range(NBLK):
                gbuild(blk)
                nxt = gen_topk(blk + 1) if blk + 1 < NBLK else None
                sweep(blk, nxt)
                if nxt is not None:
                    for _ in nxt:
                        pass
                epilogue(blk)
            S.end_phase()
        S.cur_es = es
    return nc


def host_inputs(inputs, core):
    b, s = core // 2, core % 2
    f32 = np.float32
    x = np.asarray(inputs["x"], f32)
    ctx = np.asarray(inputs["ctx"], f32)
    m = {}
    m["xf"] = np.ascontiguousarray(np.concatenate([ctx[b], x[b]], axis=0))
    m["xm"] = np.ascontiguousarray(x[b, s * HALF:(s + 1) * HALF])
    cv = np.stack([np.asarray(inputs["c"], f32)[b], np.asarray(inputs["c_ctx"], f32)], axis=-1)
    m["cvec"] = np.ascontiguousarray(cv.reshape(8, 128, 2).transpose(1, 0, 2))
    m["wmod"] = np.ascontiguousarray(np.asarray(inputs["w_mod"], f32)[0])
    bm = np.asarray(inputs["b_mod"], f32)[0]
    m["bmodT"] = np.ascontiguousarray(bm.reshape(48, 128).T)
    m["bmod"] = np.ascontiguousarray(bm.reshape(1, -1))
    ngs = np.stack([np.asarray(inputs["norm1_g"], f32)[0], np.asarray(inputs["norm2_g"], f32)[0]], axis=-1)
    m["ngT"] = np.ascontiguousarray(ngs.reshape(8, 128, 2).transpose(1, 0, 2))
    m["fng"] = np.ascontiguousarray(np.asarray(inputs["final_norm_g"], f32).reshape(1, -1))
    m["win"] = np.ascontiguousarray(np.asarray(inputs["w_in"], f32)[0])
    cw = np.asarray(inputs["conv_w"], f32)[0]
    m["convw"] = np.ascontiguousarray(cw.reshape(4, 4, 128).transpose(2, 1, 0))
    m["convb"] = np.ascontiguousarray(np.asarray(inputs["conv_b"], f32)[0].reshape(4, 128).T)
    wa = np.asarray(inputs["lru_w_a"], f32)[0]
    wx = np.asarray(inputs["lru_w_x"], f32)[0]
    lw = np.zeros((2, 2, 4, 128, 128), f32)
    for d in range(2):
        for g, w in enumerate((wa, wx)):
            for ct in range(4):
                lw[d, g, ct, 0:64, 0:64] = w[d, 2 * ct]
                lw[d, g, ct, 64:128, 64:128] = w[d, 2 * ct + 1]
    m["lruw"] = lw
    ba = np.asarray(inputs["lru_b_a"], f32)[0]
    bx = np.asarray(inputs["lru_b_x"], f32)[0]
    lb = np.stack([ba, bx], axis=1)
    m["lrub"] = np.ascontiguousarray(lb.reshape(2, 2, 4, 128).transpose(3, 0, 1, 2))
    lam = np.asarray(inputs["lru_lambda"], f32)[0]
    m["lrulam"] = np.ascontiguousarray(lam.reshape(2, 4, 128).transpose(2, 0, 1))
    gf = np.asarray(inputs["fourier_out_g"], f32)[0].reshape(4, 128).T
    gr = np.asarray(inputs["lru_out_g"], f32)[0].reshape(4, 128).T
    m["gFR"] = np.ascontiguousarray(np.concatenate([gf, gr], axis=1))
    m["wout"] = np.ascontiguousarray(np.asarray(inputs["w_out"], f32)[0])
    m["wq"] = np.ascontiguousarray(np.asarray(inputs["peer_w_q"], f32)[0])
    sk = np.asarray(inputs["peer_sub_keys"], f32)[0]
    m["keysT"] = np.ascontiguousarray(sk.reshape(16, 128, 128).transpose(2, 0, 1))
    u = np.asarray(inputs["peer_u"], f32)[0]
    m["UL"] = np.ascontiguousarray(u.reshape(128, 128, 8, 128).transpose(0, 3, 2, 1).reshape(128, 128, 1024))
    m["Vt"] = np.ascontiguousarray(np.asarray(inputs["peer_v"], f32)[0].reshape(128, 128, 1024))
    n1 = np.arange(128, dtype=np.float64)[:, None, None]
    n2 = np.arange(64, dtype=np.float64)[None, :, None]
    k1 = np.arange(128, dtype=np.float64)[None, None, :]
    ph = 2 * np.pi * (k1 * n1 / 128.0 + k1 * n2 / 8192.0)
    m["watab"] = np.ascontiguousarray(np.stack([np.cos(ph), -np.sin(ph)], axis=2).astype(f32))
    k2 = (32 * s + np.arange(32, dtype=np.float64))[None, :]
    nn = np.arange(64, dtype=np.float64)[:, None]
    ph2 = 2 * np.pi * k2 * nn / 64.0
    m["c64s"] = np.ascontiguousarray(np.stack([np.cos(ph2), np.sin(ph2), -np.sin(ph2)], axis=1).astype(f32))
    jj = np.arange(64, dtype=np.float64)[:, None]
    mm = np.arange(64, dtype=np.float64)[None, :]
    sc = 1.0 / np.sqrt(8192.0 * 64.0)
    c6 = np.cos(2 * np.pi * jj * mm / 64.0) * sc
    s6 = np.sin(2 * np.pi * jj * mm / 64.0) * sc
    bd = np.zeros((128, 2, 128))
    bd[0:64, 0, 0:64] = c6
    bd[64:128, 0, 64:128] = c6
    bd[0:64, 1, 0:64] = s6
    bd[64:128, 1, 64:128] = s6
    m["bd64"] = np.ascontiguousarray(bd.astype(f32))
    se = np.zeros((128, 2), f32)
    se[:, s] = 1.0
    m["sel"] = se
    return m


_NC_CACHE = {}


def kernel(**inputs):
    if "nc" not in _NC_CACHE:
        _NC_CACHE["nc"] = build()
    nc = _NC_CACHE["nc"]
    in_maps = [host_inputs(inputs, c) for c in range(8)]
    res = run_bass_kernel_spmd(nc, in_maps, core_ids=list(range(8)))
    outp = np.zeros((4, L, D), np.float32)
    for c in range(8):
        b, s = c // 2, c % 2
        outp[b, s * HALF:(s + 1) * HALF] = res.results[c]["out"]
    return outp
```

```python
import os
import numpy as np
import concourse.bass as bass
import concourse.mybir as mybir
from concourse.bass_utils import run_bass_kernel_spmd
from contextlib import ExitStack

F32 = mybir.dt.float32
BF16 = mybir.dt.bfloat16
U32 = mybir.dt.uint32
AF = mybir.ActivationFunctionType
ALU = mybir.AluOpType
AX = mybir.AxisListType

ENGS = ["tensor", "vector", "scalar", "gpsimd", "sync"]
EPS = 1e-6
L = 8192
CTX = 256
T = L + CTX
D = 1024
HALF = 4096
TB = 256
NCH = 128


class Buf:
    def __init__(self, t, name="", excl=False):
        self.t = t
        self.name = name
        self.excl = excl
        self.w = {}
        self.r = {}

    def __getitem__(self, idx):
        return self.t[idx]


class Sched:
    def __init__(self, nc, es):
        self.nc = nc
        self.es = es
        self.ops = {e: [] for e in ENGS}
        self.sems = {}
        for e in ["tensor", "vector", "scalar", "gpsimd"]:
            self.sems[e] = es.enter_context(nc.semaphore("c_" + e))
        self.count = {e: 0 for e in ENGS}
        self.dma_sems = {}
        self.dma_cnt = {}
        self.dma_rr = {}
        for q, n in (("sync", 32), ("gpsimd", 16), ("scalar", 8)):
            self.dma_sems[q] = [es.enter_context(nc.semaphore("d_%s%d" % (q, i))) for i in range(n)]
            self.dma_cnt[q] = [0] * n
            self.dma_rr[q] = 0
        self.waited = {}
        self.phase_dma = []
        self.cur_es = es

    def sbuf(self, name, shape, dtype):
        return Buf(self.cur_es.enter_context(self.nc.sbuf_tensor(name, list(shape), dtype)), name)

    def alias(self, buf, name=""):
        return Buf(buf.t, name)

    def psum(self, name, shape, dtype=F32):
        return Buf(self.es.enter_context(self.nc.psum_tensor(name, list(shape), dtype)), name, excl=True)

    def _deps(self, reads, writes, eng=None):
        deps = []
        for b in reads:
            deps.extend((sk, v, e) for sk, (v, e) in b.w.items())
            if b.excl:
                deps.extend((sk, v, e) for sk, (v, e) in b.r.items() if e != eng)
        for b in writes:
            deps.extend((sk, v, e) for sk, (v, e) in b.w.items())
            deps.extend((sk, v, e) for sk, (v, e) in b.r.items())
        return deps

    def _waits(self, eng, deps, skip_same_pe=True):
        waits = []
        for (sk, v, e) in deps:
            if skip_same_pe and eng == "tensor" and e == "tensor":
                continue
            key = (eng, sk)
            if self.waited.get(key, 0) >= v:
                continue
            self.waited[key] = v
            waits.append((sk, v))
        return waits

    def _semobj(self, sk):
        if isinstance(sk, tuple):
            return self.dma_sems[sk[0]][sk[1]]
        return self.sems[sk]

    def _record(self, tok, reads, writes):
        sk, v, e = tok
        for b in reads:
            if b.r.get(sk, (0, None))[0] < v:
                b.r[sk] = (v, e)
        for b in writes:
            b.w = {sk: (v, e)}
            b.r = {}

    def op(self, eng, emit, reads=(), writes=()):
        waits = self._waits(eng, self._deps(reads, writes, eng))
        self.count[eng] += 1
        tok = (eng, self.count[eng], eng)
        self.ops[eng].append((waits, emit, eng, 1))
        self._record(tok, reads, writes)
        return tok

    def dma(self, q, emit, reads=(), writes=(), persist=False):
        deps = self._deps(reads, writes)
        i = self.dma_rr[q]
        self.dma_rr[q] = (i + 1) % len(self.dma_sems[q])
        sk = (q, i)
        prev = self.dma_cnt[q][i]
        if prev > 0:
            deps.append((sk, 16 * prev, "dma"))
        waits = self._waits(q, deps)
        self.dma_cnt[q][i] = prev + 1
        tok = (sk, 16 * (prev + 1), "dma")
        self.ops[q].append((waits, emit, sk, 16))
        self._record(tok, reads, writes)
        if not persist:
            self.phase_dma.append(tok)
        return tok

    def end_phase(self):
        waits = self._waits("sync", self.phase_dma, skip_same_pe=False)
        self.ops["sync"].append((waits, None, None, 0))
        self.emit_all()
        self.ops = {e: [] for e in ENGS}
        self.phase_dma = []

    def final_wait(self, eng, bufs):
        deps = []
        for b in bufs:
            deps.extend((sk, v, e) for sk, (v, e) in b.w.items())
            deps.extend((sk, v, e) for sk, (v, e) in b.r.items())
        waits = self._waits(eng, deps, skip_same_pe=False)
        self.ops[eng].append((waits, None, None, 0))

    def emit_all(self):
        nc = self.nc
        with nc.Block() as block:
            def mk(ename):
                def body(eng):
                    for (waits, emit, sk, inc) in self.ops[ename]:
                        for (wsk, v) in waits:
                            eng.wait_ge(self._semobj(wsk), v)
                        if emit is not None:
                            inst = emit(eng)
                            inst.then_inc(self._semobj(sk), inc)
                return body
            block.sync(mk("sync"))
            block.tensor(mk("tensor"))
            block.vector(mk("vector"))
            block.scalar(mk("scalar"))
            block.gpsimd(mk("gpsimd"))


def build(stop=99, dbg=False):
    nc = bass.Bass("TRN2", target_bir_lowering=False)

    def din(name, shape, dt=F32):
        return nc.dram_tensor(name, list(shape), dt, kind="ExternalInput").ap()

    def dscr(name, shape, dt):
        return nc.dram_tensor(name, list(shape), dt, kind=("ExternalOutput" if dbg else "Internal")).ap()

    xf = din("xf", [T, D])
    xm = din("xm", [HALF, D])
    cvec = din("cvec", [128, 8, 2])
    wmod = din("wmod", [D, 6 * D])
    bmodT = din("bmodT", [128, 48])
    bmod = din("bmod", [1, 6 * D])
    ngT = din("ngT", [128, 8, 2])
    fng = din("fng", [1, D])
    win = din("win", [D, 1536])
    convw = din("convw", [128, 4, 4])
    convb = din("convb", [128, 4])
    lruw = din("lruw", [2, 2, 4, 128, 128])
    lrub = din("lrub", [128, 2, 2, 4])
    lrulam = din("lrulam", [128, 2, 4])
    gFR = din("gFR", [128, 8])
    wout = din("wout", [D, D])
    wq = din("wq", [D, 2048])
    keysT = din("keysT", [128, 16, 128])
    UL = din("UL", [NCH, 128, 1024])
    Vt = din("Vt", [NCH, 128, 1024])
    watab = din("watab", [128, 64, 2, 128])
    c64s = din("c64s", [64, 3, 32])
    bd64 = din("bd64", [128, 2, 128])
    sel = din("sel", [128, 2])
    out = nc.dram_tensor("out", [HALF, D], F32, kind="ExternalOutput").ap()

    Fd = dscr("Fd", [L, 512], BF16)
    Ud = dscr("Ud", [4, 128, T], F32)
    Gd = dscr("Gd", [4, 128, L], BF16)
    Bd = dscr("Bd", [2, 128, 64, 512], BF16)
    X1d = dscr("X1d", [HALF, D], F32)
    ULb = nc.dram_tensor("ULb", [NCH, 128, 1024], BF16, kind="Internal").ap()
    Vb = nc.dram_tensor("Vb", [NCH, 128, 1024], BF16, kind="Internal").ap()
    if dbg:
        dbgo = nc.dram_tensor("dbgo", [128, 16384], F32, kind="ExternalOutput").ap()

    es = ExitStack()
    with es:
        S = Sched(nc, es)
        dram_bufs = {}

        def dbuf(name):
            if name not in dram_bufs:
                dram_bufs[name] = Buf(None, name)
            return dram_bufs[name]

        def ACT(out_, in_, func, reads, writes, **kw):
            return S.op("scalar", lambda e: e.activation(out=out_, in_=in_, func=func, **kw), reads, writes)

        def VTS(out_, in0, s1, s2, op0, op1, reads, writes, eng="vector"):
            if s2 is None:
                return S.op(eng, lambda e: e.tensor_scalar(out=out_, in0=in0, scalar1=s1, scalar2=None, op0=op0), reads, writes)
            return S.op(eng, lambda e: e.tensor_scalar(out=out_, in0=in0, scalar1=s1, scalar2=s2, op0=op0, op1=op1), reads, writes)

        def VTT(out_, in0, in1, op, reads, writes, eng="vector"):
            return S.op(eng, lambda e: e.tensor_tensor(out=out_, in0=in0, in1=in1, op=op), reads, writes)

        def VSTT(out_, in0, sc, in1, op0, op1, reads, writes):
            return S.op("vector", lambda e: e.scalar_tensor_tensor(out=out_, in0=in0, scalar=sc, in1=in1, op0=op0, op1=op1), reads, writes)

        def VCOPY(out_, in_, reads, writes, eng="vector"):
            return S.op(eng, lambda e: e.tensor_copy(out=out_, in_=in_), reads, writes)

        def DMA(out_, in_, reads, writes, q="sync", persist=False):
            return S.dma(q, lambda e: e.dma_start(out=out_, in_=in_), reads, writes, persist=persist)

        def MM(outs, reads, writes):
            def emit(e):
                inst = None
                for (o, l, r, st, sp) in outs:
                    inst = e.matmul(o, lhsT=l, rhs=r, start=st, stop=sp)
                return inst
            return S.op("tensor", emit, reads, writes)

        def TR(outs, reads, writes):
            def emit(e):
                inst = None
                for (o, i, idn) in outs:
                    inst = e.transpose(out=o, in_=i, identity=idn)
                return inst
            return S.op("tensor", emit, reads, writes)

        PB = [S.psum("pb%d" % i, [128, 512], F32) for i in range(8)]

        ident_b = S.sbuf("ident_b", [128, 128], BF16)
        ident_f = S.sbuf("ident_f", [128, 128], F32)
        iota_f = S.sbuf("iota_f", [128, 128], F32)
        iota_b = S.sbuf("iota_b", [128, 128], BF16)
        ones_b = S.sbuf("ones_b", [128, 128], BF16)
        tmpc = S.sbuf("tmpc", [128, 128], F32)
        S.op("gpsimd", lambda e: e.iota(tmpc[:], pattern=[[1, 128]], base=0, channel_multiplier=-1,
                                        allow_small_or_imprecise_dtypes=True), [], [tmpc])
        VTS(ident_b[:], tmpc[:], 0.0, None, ALU.is_equal, None, [tmpc], [ident_b])
        VTS(ident_f[:], tmpc[:], 0.0, None, ALU.is_equal, None, [tmpc], [ident_f])
        S.op("gpsimd", lambda e: e.iota(iota_f[:], pattern=[[1, 128]], base=0, channel_multiplier=0,
                                        allow_small_or_imprecise_dtypes=True), [], [iota_f])
        VCOPY(iota_b[:], iota_f[:], [iota_f], [iota_b])
        S.op("vector", lambda e: e.memset(ones_b[:], 1.0), [], [ones_b])

        cv = S.sbuf("cv", [128, 8, 2], F32)
        scv = S.sbuf("scv", [128, 8, 2], F32)
        bmT = S.sbuf("bmT", [128, 48], F32)
        ng = S.sbuf("ng", [128, 8, 2], F32)
        modT = S.sbuf("modT", [128, 48, 2], F32)
        g2B = S.sbuf("g2B", [128, D], F32)
        fngB = S.sbuf("fngB", [128, D], F32)
        selt = S.sbuf("selt", [128, 2], F32)
        A1x = S.sbuf("A1x", [128, 8], F32)
        A1c = S.sbuf("A1c", [128, 8], F32)
        A2x = S.sbuf("A2x", [128, 8], F32)
        xt = [S.sbuf("xt%d" % i, [128, D], F32) for i in range(3)]
        xs = [S.sbuf("xs%d" % i, [128, D], BF16) for i in range(2)]
        junk = S.sbuf("junk", [128, D], BF16)
        ssq = [S.sbuf("ssq%d" % i, [128, 1], F32) for i in range(4)]
        rstd = [S.sbuf("rstd%d" % i, [128, 1], F32) for i in range(4)]
        cnt = {"xt": 0, "xs": 0, "ss": 0}
        g1_es = ExitStack()
        S.cur_es = g1_es
        g1B = S.sbuf("g1B", [128, D], F32)
        S.cur_es = es
        DMA(cv[:], cvec, [], [cv])
        DMA(bmT[:], bmodT, [], [bmT])
        DMA(ng[:], ngT, [], [ng])
        DMA(selt[:], sel, [], [selt])
        DMA(fngB[:], fng[0:1, :].partition_broadcast(128), [], [fngB])
        DMA(g1B[:], bmod[0:1, 2 * D:3 * D].partition_broadcast(128), [], [g1B])
        DMA(g2B[:], bmod[0:1, 5 * D:6 * D].partition_broadcast(128), [], [g2B])
        ACT(scv[:], cv[:], AF.Silu, [cv], [scv])

        def AP3(buf, rowlen, off, dims):
            return bass.AP(tensor=buf.t, offset=off, ap=[[rowlen, 128]] + [list(d) for d in dims])

        def phase_scope():
            ph = ExitStack()
            S.cur_es = ph
            return ph

        with phase_scope():
            wm = [S.sbuf("wm%d" % i, [128, 8, 512], F32) for i in range(2)]
            screp = S.sbuf("screp", [128, 8, 128], F32)
            for j in range(8):
                VCOPY(screp[:, j, :], scv[:, j, 0:1].to_broadcast([128, 128]), [scv], [screp])
            wmod_v = wmod.rearrange("(j p) c -> p j c", p=128)
            for grp in range(12):
                wmb = wm[grp % 2]
                DMA(wmb[:], wmod_v[:, :, grp * 512:(grp + 1) * 512], [], [wmb])
                pb = PB[grp % 2]
                outs = []
                for o4 in range(4):
                    for j in range(8):
                        outs.append((pb[:, o4 * 2:o4 * 2 + 2], wmb[:, j, o4 * 128:(o4 + 1) * 128], scv[:, j, :], j == 0, j == 7))
                MM(outs, [wmb, scv], [pb])
                oc0 = grp * 4
                for xc in range(2):
                    VTT(modT[:, oc0:oc0 + 4, xc], pb[:, xc:8:2], bmT[:, oc0:oc0 + 4], ALU.add, [pb, bmT], [modT])
                if grp in (4, 5, 10, 11):
                    pb2 = PB[2 + grp % 2]
                    outs = [(pb2[:, :], screp[:, j, :], wmb[:, j, :], j == 0, j == 7) for j in range(8)]
                    MM(outs, [wmb, screp], [pb2])
                    gB = g1B if grp < 6 else g2B
                    cs = (grp % 2) * 512
                    VTT(gB[:, cs:cs + 512], gB[:, cs:cs + 512], pb2[:, :], ALU.add, [pb2], [gB])
            VSTT(A1x[:], modT[:, 8:16, 0], 1.0, ng[:, :, 0], ALU.add, ALU.mult, [modT, ng], [A1x])
            VSTT(A1c[:], modT[:, 8:16, 1], 1.0, ng[:, :, 0], ALU.add, ALU.mult, [modT, ng], [A1c])
            VSTT(A2x[:], modT[:, 32:40, 0], 1.0, ng[:, :, 1], ALU.add, ALU.mult, [modT, ng], [A2x])
            S.end_phase()
        S.cur_es = es


        def rms_rstd(src_ap, src_buf, width):
            k = cnt["ss"] % 4
            cnt["ss"] += 1
            ACT(junk[:, 0:width], src_ap, AF.Square, [src_buf], [junk, ssq[k]], accum_out=ssq[k][:])
            ACT(rstd[k][:], ssq[k][:], AF.Sqrt, [ssq[k]], [rstd[k]], scale=1.0 / width, bias=EPS)
            S.op("vector", lambda e: e.reciprocal(out=rstd[k][:], in_=rstd[k][:]), [], [rstd[k]])
            return rstd[k]

        def norm_transpose(xb, banks, col0):
            r = rms_rstd(xb[:], xb, D)
            xsb = xs[cnt["xs"] % 2]
            cnt["xs"] += 1
            ACT(xsb[:], xb[:], AF.Copy, [xb, r], [xsb], scale=r[:])
            for bk in range(4):
                outs = []
                for jj in range(2):
                    j = bk * 2 + jj
                    o = banks[bk][:, :].bitcast(BF16)[:, jj * 512 + col0: jj * 512 + col0 + 128]
                    outs.append((o, xsb[:, j * 128:(j + 1) * 128], ident_b[:]))
                TR(outs, [xsb, ident_b], [banks[bk]])

        def load_x(src_rows):
            xb = xt[cnt["xt"] % 3]
            cnt["xt"] += 1
            DMA(xb[:], src_rows, [], [xb])
            return xb

        Fd_parts, Ud_parts, Gd_parts, UV_parts = [], [], [], []
        with phase_scope():
            winb = S.sbuf("winb", [128, 8, 1536], BF16)
            DMA(winb[:], win.rearrange("(j p) c -> p j c", p=128), [], [winb], q="gpsimd")
            hxT = [S.sbuf("hxT%d" % i, [128, 8, 512], BF16) for i in range(2)]
            fsb = [S.sbuf("fsb%d" % i, [128, 512], BF16) for i in range(2)]
            usb = [S.sbuf("usb%d" % i, [128, 512], F32) for i in range(2)]
            gsb = [S.sbuf("gsb%d" % i, [128, 512], BF16) for i in range(2)]
            k_f = k_u = k_g = k_p = 0
            xs1a = [S.sbuf("xs1a%d" % i, [128, D], BF16) for i in range(8)]

            def blk_geom(blk):
                if blk == 0:
                    return 0, 2, A1c, 1
                return CTX + (blk - 1) * 512, 4, A1x, 0

            def norm_part(blk):
                tok0, ntile, _, _ = blk_geom(blk)
                tiles = []
                for tt in range(ntile):
                    xb = load_x(xf[tok0 + tt * 128: tok0 + (tt + 1) * 128, :])
                    r = rms_rstd(xb[:], xb, D)
                    xsb = xs1a[(blk % 2) * 4 + tt]
                    ACT(xsb[:], xb[:], AF.Copy, [xb, r], [xsb], scale=r[:])
                    tiles.append(xsb)
                return tiles

            nxt_tiles = norm_part(0)
            for blk in range(17):
                tok0, ntile, Am, Sm_col = blk_geom(blk)
                ntok = ntile * 128
                hb = hxT[blk % 2]
                cur_tiles = nxt_tiles
                for tt in range(ntile):
                    xsb = cur_tiles[tt]
                    for bk in range(4):
                        outs = []
                        for jj in range(2):
                            j = bk * 2 + jj
                            o = PB[bk][:, :].bitcast(BF16)[:, jj * 512 + tt * 128: jj * 512 + tt * 128 + 128]
                            outs.append((o, xsb[:, j * 128:(j + 1) * 128], ident_b[:]))
                        TR(outs, [xsb, ident_b], [PB[bk]])
                if blk + 1 < 17:
                    nxt_tiles = norm_part(blk + 1)
                for j in range(8):
                    src = PB[j // 2][:, :].bitcast(BF16)[:, (j % 2) * 512:(j % 2) * 512 + ntok]
                    VTS(hb[:, j, 0:ntok], src, Am[:, j:j + 1], modT[:, j, Sm_col:Sm_col + 1], ALU.mult, ALU.add,
                        [PB[j // 2], Am, modT], [hb])
                if blk > 0:
                    lat0 = tok0 - CTX
                    for tt in range(ntile):
                        pb = PB[4 + k_p % 2]
                        k_p += 1
                        MM([(pb[:, :], hb[:, j, tt * 128:(tt + 1) * 128], winb[:, j, 0:512], j == 0, j == 7) for j in range(8)],
                           [hb, winb], [pb])
                        fb = fsb[k_f % 2]
                        k_f += 1
                        VCOPY(fb[:], pb[:, :], [pb], [fb])
                        part = Buf(None)
                        Fd_parts.append(part)
                        DMA(Fd[lat0 + tt * 128: lat0 + (tt + 1) * 128, :], fb[:], [fb], [part], q="gpsimd")
                for ct in range(8):
                    if ct >= 4 and blk == 0:
                        continue
                    pb = PB[6 + k_p % 2]
                    k_p += 1
                    MM([(pb[:, 0:ntok], winb[:, j, 512 + ct * 128: 512 + (ct + 1) * 128], hb[:, j, 0:ntok], j == 0, j == 7) for j in range(8)],
                       [hb, winb], [pb])
                    if ct < 4:
                        ub = usb[k_u % 2]
                        k_u += 1
                        VCOPY(ub[:, 0:ntok], pb[:, 0:ntok], [pb], [ub])
                        part = Buf(None)
                        Ud_parts.append(part)
                        DMA(Ud[ct, :, tok0:tok0 + ntok], ub[:, 0:ntok], [ub], [part], q="gpsimd")
                    else:
                        gb = gsb[k_g % 2]
                        k_g += 1
                        ACT(gb[:, 0:ntok], pb[:, 0:ntok], AF.Gelu_apprx_tanh, [pb], [gb])
                        part = Buf(None)
                        Gd_parts.append(part)
                        DMA(Gd[ct - 4, :, tok0 - CTX: tok0 - CTX + ntok], gb[:, 0:ntok], [gb], [part], q="gpsimd")
            S.end_phase()
        S.cur_es = es
        if stop <= 1:
            g1_es.close()
            return nc

        mix_es = ExitStack()
        S.cur_es = mix_es
        RX = [S.sbuf("RX%d" % i, [128, HALF], BF16) for i in range(4)]

        with phase_scope():
            CHW = 1024
            NLC = L // CHW
            HF = S.sbuf("HF", [128, L], F32)
            HFb = [HF, HF]
            wk = {}
            for nm in ("UU", "UC", "R", "Q", "H", "HB"):
                wk[(0, nm)] = [S.sbuf("%s_%d" % (nm, i), [128, CHW], F32) for i in range(2)]
                wk[(1, nm)] = wk[(0, nm)]
            GGb = [S.sbuf("GGb%d" % i, [128, CHW], BF16) for i in range(2)]
            lw = S.sbuf("lw", [128, 16, 128], F32)
            cw = S.sbuf("cw", [128, 4, 4], F32)
            cb = S.sbuf("cb", [128, 4], F32)
            lb = S.sbuf("lb", [128, 2, 2, 4], F32)
            lam = S.sbuf("lam", [128, 2, 4], F32)
            sp8 = S.sbuf("sp8", [128, 2, 4], F32)
            sp16 = S.sbuf("sp16", [128, 2, 4], F32)
            DMA(lw[:], lruw.rearrange("d g c k m -> k (d g c) m"), [], [lw])
            conv_jobs = []
            if stop > 4:
                for c8 in range(32):
                    for src_, dst_ in ((UL, ULb), (Vt, Vb)):
                        conv_jobs.append((dst_[c8 * 4:(c8 + 1) * 4], src_[c8 * 4:(c8 + 1) * 4]))
            conv_jobs.reverse()

            def conv_step(gate):
                if conv_jobs:
                    dst_, src_ = conv_jobs.pop()
                    part = Buf(None)
                    UV_parts.append(part)
                    DMA(dst_, src_, [gate], [part], q="gpsimd", persist=True)
            DMA(cw[:], convw, [], [cw])
            DMA(cb[:], convb, [], [cb])
            DMA(lb[:], lrub, [], [lb])
            DMA(lam[:], lrulam, [], [lam])
            ACT(sp8[:], lam[:], AF.Exp, [lam], [sp8], scale=-1.0)
            ACT(sp8[:], sp8[:], AF.Ln, [], [sp8], bias=1.0)
            VTS(sp16[:], sp8[:], -16.0, None, ALU.mult, None, [sp8], [sp16])
            VTS(sp8[:], sp8[:], -8.0, None, ALU.mult, None, [], [sp8])
            chunks = [(0, CTX)] + [(CTX + k * CHW, CHW) for k in range(NLC)]
            kk = {"p": 0, "w0": 0, "w1": 0, "g": 0}

            def lru_pass(ct, d):
                HFc = HFb[ct % 2]
                order = list(range(NLC + 1)) if d == 0 else [0] + list(range(NLC, 0, -1))
                st = {"carry": None}
                held = {}

                def stage_a(ci):
                    c0, cl = chunks[ci]
                    w = kk["w0"] % 2
                    kk["w0"] += 1
                    UU, UC, R, Q, H = (wk[(d, nm)][w] for nm in ("UU", "UC", "R", "Q", "H"))
                    HB = wk[(1, "HB")][w] if d == 1 else None
                    held[ci] = (c0, cl, UU, UC, R, Q, H, HB)
                    DMA(UU[:, 0:cl], Ud[ct, :, c0:c0 + cl], Ud_parts, [UU])
                    gate = Buf(None)
                    ACT(UC[:, 0:cl], UU[:, 0:cl], AF.Identity, [UU, cw, cb], [UC, gate], scale=cw[:, ct, 2:3], bias=cb[:, ct:ct + 1])
                    conv_step(gate)
                    for k, o in ((0, -2), (1, -1), (3, 1)):
                        a = abs(o)
                        if ci == 0:
                            if o < 0:
                                oap, iap = UC[:, a:cl], UU[:, 0:cl - a]
                            else:
                                oap, iap = UC[:, 0:cl - a], UU[:, a:cl]
                        else:
                            nr = cl // 64
                            if o < 0:
                                oap = AP3(UC, CHW, a, [[64, nr], [1, 64 - a]])
                                iap = AP3(UU, CHW, 0, [[64, nr], [1, 64 - a]])
                            else:
                                oap = AP3(UC, CHW, 0, [[64, nr], [1, 64 - a]])
                                iap = AP3(UU, CHW, a, [[64, nr], [1, 64 - a]])
                        VSTT(oap, iap, cw[:, ct, k:k + 1], oap, ALU.mult, ALU.add, [UU, cw], [UC])
                    sw = min(512, cl)
                    for sub in range(cl // sw):
                        for g, dst in ((0, R), (1, H)):
                            pb = PB[kk["p"] % 8]
                            kk["p"] += 1
                            MM([(pb[:, 0:sw], lw[:, (d * 2 + g) * 4 + ct, :], UC[:, sub * sw:(sub + 1) * sw], True, True)], [lw, UC], [pb])
                            ACT(dst[:, sub * sw:(sub + 1) * sw], pb[:, 0:sw], AF.Sigmoid, [pb, lb], [dst], bias=lb[:, d, g, ct:ct + 1])

                def stage_b(ci):
                    c0, cl, UU, UC, R, Q, H, HB = held.pop(ci)
                    carry = st["carry"]
                    ACT(Q[:, 0:cl], R[:, 0:cl], AF.Exp, [R, sp16], [Q], scale=sp16[:, d, ct:ct + 1])
                    ACT(R[:, 0:cl], R[:, 0:cl], AF.Exp, [sp8], [R], scale=sp8[:, d, ct:ct + 1])
                    ACT(Q[:, 0:cl], Q[:, 0:cl], AF.Sqrt, [], [Q], scale=-1.0, bias=1.0)
                    VTT(H[:, 0:cl], H[:, 0:cl], UC[:, 0:cl], ALU.mult, [UC], [H])
                    VTT(H[:, 0:cl], H[:, 0:cl], Q[:, 0:cl], ALU.mult, [Q], [H])
                    init = 0.0 if carry is None else carry[1]
                    rds = [R, H] + ([] if carry is None else [carry[0]])
                    if d == 0:
                        if ci == 0:
                            ob, oap, cap = Q, Q[:, 0:cl], Q[:, cl - 1:cl]
                        else:
                            l0 = c0 - CTX
                            ob, oap, cap = HFc, HFc[:, l0:l0 + cl], HFc[:, l0 + cl - 1:l0 + cl]
                        S.op("vector", lambda e, oap=oap, R=R, H=H, init=init, cl=cl: e.tensor_tensor_scan(
                            out=oap, data0=R[:, 0:cl], data1=H[:, 0:cl], initial=init, op0=ALU.mult, op1=ALU.add), rds, [ob])
                        st["carry"] = (ob, cap)
                    else:
                        ob = Q if ci == 0 else HB
                        S.op("vector", lambda e, ob=ob, R=R, H=H, init=init, cl=cl: e.tensor_tensor_scan(
                            out=ob[:, cl - 1::-1], data0=R[:, cl - 1::-1], data1=H[:, cl - 1::-1], initial=init,
                            op0=ALU.mult, op1=ALU.add), rds, [ob])
                        st["carry"] = (ob, ob[:, 0:1])
                        if ci > 0:
                            l0 = c0 - CTX
                            GG = GGb[kk["g"] % 2]
                            kk["g"] += 1
                            DMA(GG[:], Gd[ct, :, l0:l0 + cl], Gd_parts, [GG])
                            VTT(H[:], HB[:], HFc[:, l0:l0 + cl], ALU.add, [HB, HFc], [H])
                            VTT(H[:], H[:], GG[:], ALU.mult, [GG], [H])
                            half, pos0 = (ci - 1) // (NLC // 2), ((ci - 1) % (NLC // 2)) * CHW
                            if half == 1:
                                VTS(RX[ct][:, pos0:pos0 + cl], H[:], selt[:, 1:2], None, ALU.mult, None, [H, selt], [RX[ct]])
                            else:
                                VSTT(RX[ct][:, pos0:pos0 + cl], H[:], selt[:, 0:1], RX[ct][:, pos0:pos0 + cl],
                                     ALU.mult, ALU.add, [H, selt], [RX[ct]])

                stage_a(order[0])
                for idx, ci in enumerate(order):
                    if idx + 1 < len(order):
                        stage_a(order[idx + 1])
                    stage_b(ci)
                    yield

            for ct in range(4):
                for d in range(2):
                    for _ in lru_pass(ct, d):
                        pass
            while conv_jobs:
                conv_step(Buf(None))
            if dbg:
                for ct in range(4):
                    VCOPY(HF[:, (ct % 2) * 4096:(ct % 2) * 4096 + 4096], RX[ct][:], [RX[ct]], [HF])
                    DMA(dbgo[:, ct * 4096:(ct + 1) * 4096], HF[:, (ct % 2) * 4096:(ct % 2) * 4096 + 4096], [HF], [Buf(None)])
            S.end_phase()
        S.cur_es = es
        if stop <= 2:
            mix_es.close()
            g1_es.close()
            return nc

        S.cur_es = mix_es
        YT = [S.sbuf("YT%d" % i, [128, HALF], BF16) for i in range(4)]
        Bd_parts = []
        with phase_scope():
            WA = S.sbuf("WA", [128, 64 * 2 * 128], BF16)
            for q4 in range(4):
                DMA(WA[:, q4 * 4096:(q4 + 1) * 4096], watab[:, q4 * 16:(q4 + 1) * 16, :, :].rearrange("p a b c -> p (a b c)"),
                    [], [WA], q="gpsimd")
            fa = [S.sbuf("fa%d" % i, [128, 512], BF16) for i in range(3)]
            bsb = [S.sbuf("bsb%d" % i, [128, 2, 512], BF16) for i in range(2)]
            Fd_v = Fd.rearrange("(a b) c -> a b c", b=64)
            for n2 in range(64):
                f = fa[n2 % 3]
                DMA(f[:], Fd_v[:, n2, :], Fd_parts, [f])
                pr, pi = PB[(n2 % 2) * 2], PB[(n2 % 2) * 2 + 1]
                MM([(pr[:, :], WA[:, (n2 * 2 + 0) * 128:(n2 * 2 + 1) * 128], f[:], True, True)], [WA, f], [pr])
                MM([(pi[:, :], WA[:, (n2 * 2 + 1) * 128:(n2 * 2 + 2) * 128], f[:], True, True)], [WA, f], [pi])
                bs = bsb[n2 % 2]
                ACT(bs[:, 0, :], pr[:, :], AF.Copy, [pr], [bs])
                VCOPY(bs[:, 1, :], pi[:, :], [pi], [bs])
                part = Buf(None)
                Bd_parts.append(part)
                DMA(Bd[:, :, n2, :].rearrange("r k c -> k r c"), bs[:], [bs], [part], q="gpsimd")
            S.end_phase()
        S.cur_es = es
        with phase_scope():
            C64 = S.sbuf("C64", [64, 3, 32], BF16)
            BD = S.sbuf("BD", [128, 2, 128], BF16)
            DMA(C64[:], c64s, [], [C64], q="gpsimd")
            DMA(BD[:], bd64, [], [BD], q="gpsimd")
            XT = [[[S.sbuf("XT%d_%d_%d" % (w_, ri, cc), [128, 512], BF16) for cc in range(4)] for ri in range(2)] for w_ in range(2)]
            bt = [S.sbuf("bt%d" % i, [64, 2, 512], BF16) for i in range(3)]
            for k1 in range(128):
                b_ = bt[k1 % 3]
                DMA(b_[:], Bd[:, k1, :, :].rearrange("r n c -> n r c"), Bd_parts, [b_])
                g, pos = k1 // 16, k1 % 16
                for cc in range(4):
                    cs = slice(cc * 128, (cc + 1) * 128)
                    MM([(PB[cc][:, pos * 32:(pos + 1) * 32], b_[:, 0, cs], C64[:, 0, :], True, False),
                        (PB[cc][:, pos * 32:(pos + 1) * 32], b_[:, 1, cs], C64[:, 1, :], False, True)], [b_, C64], [PB[cc]])
                    MM([(PB[4 + cc][:, pos * 32:(pos + 1) * 32], b_[:, 1, cs], C64[:, 0, :], True, False),
                        (PB[4 + cc][:, pos * 32:(pos + 1) * 32], b_[:, 0, cs], C64[:, 2, :], False, True)], [b_, C64], [PB[4 + cc]])
                if pos == 15:
                    X = XT[g % 2]
                    ts_ = slice(g * 512, (g + 1) * 512)
                    for cc in range(4):
                        ACT(X[0][cc][:], PB[cc][:, :], AF.Copy, [PB[cc]], [X[0][cc]])
                        VCOPY(X[1][cc][:], PB[4 + cc][:, :], [PB[4 + cc]], [X[1][cc]])
                    for cc in range(4):
                        pb = PB[cc]
                        MM([(pb[:, :], BD[:, 0, :], X[0][cc][:], True, False),
                            (pb[:, :], BD[:, 1, :], X[1][cc][:], False, True)], [BD, X[0][cc], X[1][cc]], [pb])
                        if cc % 2 == 0:
                            ACT(YT[cc][:, ts_], pb[:, :], AF.Copy, [pb], [YT[cc]])
                        else:
                            VCOPY(YT[cc][:, ts_], pb[:, :], [pb], [YT[cc]])
            if dbg and stop == 3:
                for cc in range(4):
                    dtmp = S.sbuf("dtmp%d" % cc, [128, HALF], F32)
                    VCOPY(dtmp[:], YT[cc][:], [YT[cc]], [dtmp])
                    DMA(dbgo[:, cc * 4096:(cc + 1) * 4096], dtmp[:], [dtmp], [Buf(None)])
            S.end_phase()
        S.cur_es = es
        if stop <= 3:
            mix_es.close()
            g1_es.close()
            return nc

        X1_parts = []
        with phase_scope():
            gfr = S.sbuf("gfr", [128, 8], F32)
            DMA(gfr[:], gFR, [], [gfr])
            woutb = S.sbuf("woutb", [128, 8, D], BF16)
            DMA(woutb[:], wout.rearrange("(j p) c -> p j c", p=128), [], [woutb], q="gpsimd")
            for j in range(8):
                VTT(woutb[:, j, :], woutb[:, j, :], g1B[:], ALU.mult, [g1B], [woutb])
            sq = [S.sbuf("sq%d" % i, [128, 512], BF16) for i in range(2)]
            rs = [S.sbuf("rs%d" % i, [128, 512], F32) for i in range(2)]
            kq = 0
            for gi, grp in enumerate((YT, RX)):
                for tb in range(8):
                    ts_ = slice(tb * 512, (tb + 1) * 512)
                    pb = PB[tb % 2]
                    for cc in range(4):
                        q_ = sq[kq % 2]
                        kq += 1
                        ACT(q_[:], grp[cc][:, ts_], AF.Square, [grp[cc]], [q_])
                        MM([(pb[:, :], ones_b[:], q_[:], cc == 0, cc == 3)], [ones_b, q_], [pb])
                    r_ = rs[tb % 2]
                    ACT(r_[:], pb[:, :], AF.Sqrt, [pb], [r_], scale=1.0 / 512, bias=EPS)
                    S.op("vector", lambda e, r_=r_: e.reciprocal(out=r_[:], in_=r_[:]), [], [r_])
                    for cc in range(4):
                        VSTT(grp[cc][:, ts_], grp[cc][:, ts_], gfr[:, gi * 4 + cc: gi * 4 + cc + 1], r_[:], ALU.mult, ALU.mult,
                             [r_, gfr], [grp[cc]])
            x1sb = [S.sbuf("x1sb%d" % i, [128, D], F32) for i in range(2)]
            for tt in range(32):
                xb = load_x(xm[tt * 128:(tt + 1) * 128, :])
                x1 = x1sb[tt % 2]
                for dh in range(2):
                    pb = PB[2 + (tt % 2) * 2 + dh]
                    outs = []
                    for cc in range(4):
                        outs.append((pb[:, :], YT[cc][:, tt:HALF:32], woutb[:, cc, dh * 512:(dh + 1) * 512], cc == 0, False))
                    for cc in range(4):
                        outs.append((pb[:, :], RX[cc][:, tt * 128:(tt + 1) * 128], woutb[:, 4 + cc, dh * 512:(dh + 1) * 512], False, cc == 3))
                    MM(outs, YT + RX + [woutb], [pb])
                    VTT(x1[:, dh * 512:(dh + 1) * 512], pb[:, :], xb[:, dh * 512:(dh + 1) * 512], ALU.add, [pb, xb], [x1])
                part = Buf(None)
                X1_parts.append(part)
                DMA(X1d[tt * 128:(tt + 1) * 128, :], x1[:], [x1], [part], q="gpsimd")
            S.end_phase()
        S.cur_es = es
        mix_es.close()
        g1_es.close()
        if stop <= 4:
            return nc

        out_parts = []
        with phase_scope():
            wqb = S.sbuf("wqb", [128, 8, 2048], BF16)
            DMA(wqb[:], wq.rearrange("(j p) c -> p j c", p=128), [], [wqb], q="gpsimd")
            kT = S.sbuf("kT", [128, 16, 128], BF16)
            DMA(kT[:], keysT, [], [kT], q="gpsimd")
            G_all = S.sbuf("G_all", [128, TB, 128], BF16)
            h2Tb = [S.sbuf("h2T%d" % i, [128, 8, TB], BF16) for i in range(2)]
            qT = S.sbuf("qT", [128, 16, TB], BF16)
            S_sb = S.sbuf("S_sb", [128, 2048], F32)
            S2big = S.sbuf("S2big", [128, 2048], F32)
            S2s = [S.alias(S2big) for i in range(16)]
            c2s = [S.alias(S2big) for i in range(8)]

            def S2ap(hp):
                return S2big[:, hp * 128:(hp + 1) * 128]

            def c2ap(h):
                return S2big[:, h * 256:(h + 1) * 256]
            Vv = S.sbuf("Vv", [128, 8, 2, 16], F32)
            Iu = S.sbuf("Iu", [128, 8, 2, 16], U32)
            If_ = S.sbuf("If", [128, 8, 2, 16], F32)
            cand = S.sbuf("cand", [128, 8, 256], F32)
            SC = S.sbuf("SC", [128, 8, 16], F32)
            Pu = S.sbuf("Pu", [128, 8, 16], U32)
            PA = S.sbuf("PA", [128, 8, 16], U32)
            PBu = S.sbuf("PBu", [128, 8, 16], U32)
            PAf = S.sbuf("PAf", [128, 128], F32)
            PBf = S.sbuf("PBf", [128, 128], F32)
            Ee = S.sbuf("Ee", [128, 8, 16], F32)
            Vv_a = [S.alias(Vv) for _ in range(16)]
            Vv_b = [S.alias(Vv) for _ in range(16)]
            Iu_a = [S.alias(Iu) for _ in range(16)]
            Iu_b = [S.alias(Iu) for _ in range(16)]
            SC_a = [S.alias(SC) for _ in range(8)]
            SC_b = [S.alias(SC) for _ in range(8)]
            Pu_a = [S.alias(Pu) for _ in range(8)]
            Pu_b = [S.alias(Pu) for _ in range(8)]
            Zs = S.sbuf("Zs", [128, 8], F32)
            I12G = [S.sbuf("I12G%d" % i, [128, 128], F32) for i in range(3)]
            IGT = [S.sbuf("IGT%d" % i, [128, TB], F32) for i in range(3)]
            NOH = 4
            lt = [S.sbuf("lt%d" % i, [128, 128], BF16) for i in range(NOH)]
            rt = [S.sbuf("rt%d" % i, [128, 128], BF16) for i in range(NOH)]
            NUT, NVC, NGA, NWT = 4, 4, 2, 3
            UTb = [S.sbuf("UTb%d" % i, [128, 8, 128], BF16) for i in range(NUT)]
            Vcb = [S.sbuf("Vcb%d" % i, [128, D], BF16) for i in range(NVC)]
            gab = [S.sbuf("gab%d" % i, [128, TB], BF16) for i in range(NGA)]
            Wtb = [S.sbuf("Wtb%d" % i, [128, TB], BF16) for i in range(NWT)]
            NTT = TB // 128
            NBLK = HALF // TB
            if dbg and stop == 5:
                NBLK = 2
            PQ = PB[7]

            def gen_topk(blk):
                h2T = h2Tb[blk % 2]
                for tt in range(NTT):
                    r0 = blk * TB + tt * 128
                    xb = load_x(X1d[r0:r0 + 128, :])
                    r = rms_rstd(xb[:], xb, D)
                    yield
                    xsb = xs[cnt["xs"] % 2]
                    cnt["xs"] += 1
                    ACT(xsb[:], xb[:], AF.Copy, [xb, r], [xsb], scale=r[:])
                    yield
                    yield
                    pq16 = PQ[:, :].bitcast(BF16)
                    TR([(pq16[:, j * 128:(j + 1) * 128], xsb[:, j * 128:(j + 1) * 128], ident_b[:]) for j in range(8)], [xsb, ident_b], [PQ])
                    yield
                    for j in range(8):
                        VTS(h2T[:, j, tt * 128:(tt + 1) * 128], pq16[:, j * 128:(j + 1) * 128], A2x[:, j:j + 1], modT[:, 24 + j, 0:1],
                            ALU.mult, ALU.add, [PQ, A2x, modT], [h2T])
                    yield
                    yield
                for hp2 in range(8):
                    outs = []
                    for q in range(2):
                        hp = hp2 * 2 + q
                        outs += [(PQ[:, q * TB:(q + 1) * TB], wqb[:, j, hp * 128:(hp + 1) * 128], h2T[:, j, :], j == 0, j == 7) for j in range(8)]
                    MM(outs, [wqb, h2T], [PQ])
                    yield
                    VCOPY(qT[:, hp2 * 2:hp2 * 2 + 2, :], PQ[:, 0:2 * TB].rearrange("p (q t) -> p q t", q=2), [PQ], [qT])
                    yield
                for tt in range(NTT):
                    tsl = slice(tt * 128, (tt + 1) * 128)
                    for b4 in range(4):
                        MM([(PQ[:, q * 128:(q + 1) * 128], qT[:, b4 * 4 + q, tsl], kT[:, b4 * 4 + q, :], True, True) for q in range(4)],
                           [qT, kT], [PQ])
                        yield
                        VCOPY(S_sb[:, b4 * 512:(b4 + 1) * 512], PQ[:, :], [PQ], [S_sb])
                        yield
                    hps = [(hp, hp // 2, hp % 2, S_sb[:, hp * 128:(hp + 1) * 128]) for hp in range(16)]
                    for hp, h_, p_, srow in hps:
                        S.op("vector", lambda e, h_=h_, p_=p_, srow=srow: e.max(out=Vv[:, h_, p_, 0:8], in_=srow), [S_sb], [Vv_a[hp]])
                    yield
                    for hp, h_, p_, srow in hps:
                        S.op("vector", lambda e, h_=h_, p_=p_, srow=srow: e.max_index(out=Iu[:, h_, p_, 0:8], in_max=Vv[:, h_, p_, 0:8], in_values=srow), [S_sb, Vv_a[hp]], [Iu_a[hp]])
                    yield
                    for hp, h_, p_, srow in hps:
                        S.op("vector", lambda e, hp=hp, h_=h_, p_=p_, srow=srow: e.match_replace(out=S2ap(hp), in_to_replace=Vv[:, h_, p_, 0:8], in_values=srow, imm_value=-1e30), [S_sb, Vv_a[hp]], [S2s[hp], c2s[hp // 2]])
                    yield
                    for hp, h_, p_, srow in hps:
                        S.op("vector", lambda e, hp=hp, h_=h_, p_=p_: e.max(out=Vv[:, h_, p_, 8:16], in_=S2ap(hp)), [S2s[hp]], [Vv_b[hp]])
                    yield
                    for hp, h_, p_, srow in hps:
                        S.op("vector", lambda e, hp=hp, h_=h_, p_=p_: e.max_index(out=Iu[:, h_, p_, 8:16], in_max=Vv[:, h_, p_, 8:16], in_values=S2ap(hp)), [S2s[hp], Vv_b[hp]], [Iu_b[hp]])
                    yield
                    VCOPY(If_[:], Iu[:], Iu_a + Iu_b, [If_])
                    cand4 = cand[:, :, :].rearrange("p h (a b) -> p h a b", b=16)
                    VTT(cand4, Vv[:, :, 0, :].unsqueeze(3).to_broadcast([128, 8, 16, 16]),
                        Vv[:, :, 1, :].unsqueeze(2).to_broadcast([128, 8, 16, 16]), ALU.add, Vv_a + Vv_b, [cand])
                    yield
                    for h_ in range(8):
                        S.op("vector", lambda e, h_=h_: e.max(out=SC[:, h_, 0:8], in_=cand[:, h_, :]), [cand], [SC_a[h_]])
                    yield
                    for h_ in range(8):
                        S.op("vector", lambda e, h_=h_: e.max_index(out=Pu[:, h_, 0:8], in_max=SC[:, h_, 0:8], in_values=cand[:, h_, :]), [cand, SC_a[h_]], [Pu_a[h_]])
                    for h_ in range(8):
                        S.op("vector", lambda e, h_=h_: e.match_replace(out=c2ap(h_), in_to_replace=SC[:, h_, 0:8], in_values=cand[:, h_, :], imm_value=-1e30), [cand, SC_a[h_]], [c2s[h_], S2s[2 * h_], S2s[2 * h_ + 1]])
                    yield
                    for h_ in range(8):
                        S.op("vector", lambda e, h_=h_: e.max(out=SC[:, h_, 8:16], in_=c2ap(h_)), [c2s[h_]], [SC_b[h_]])
                    yield
                    for h_ in range(8):
                        S.op("vector", lambda e, h_=h_: e.max_index(out=Pu[:, h_, 8:16], in_max=SC[:, h_, 8:16], in_values=c2ap(h_)), [c2s[h_], SC_b[h_]], [Pu_b[h_]])
                    VTT(Ee[:], SC[:], SC[:, :, 0:1].to_broadcast([128, 8, 16]), ALU.subtract, SC_a + SC_b, [Ee])
                    yield
                    ACT(Ee[:], Ee[:], AF.Exp, [], [Ee])
                    VTS(PA[:], Pu[:], 4, None, ALU.logical_shift_right, None, Pu_a + Pu_b, [PA])
                    VTS(PBu[:], Pu[:], 15, None, ALU.bitwise_and, None, Pu_a + Pu_b, [PBu])
                    yield
                    VCOPY(PAf[:, :].rearrange("p (h k) -> p h k", k=16), PA[:], [PA], [PAf])
                    VCOPY(PBf[:, :].rearrange("p (h k) -> p h k", k=16), PBu[:], [PBu], [PBf])
                    S.op("vector", lambda e: e.tensor_reduce(out=Zs[:], in_=Ee[:], axis=AX.X, op=ALU.add), [Ee], [Zs])
                    yield
                    S.op("vector", lambda e: e.reciprocal(out=Zs[:], in_=Zs[:]), [], [Zs])
                    yield
                    GW = I12G[2]
                    VTT(GW[:, :].rearrange("p (h k) -> p h k", k=16), Ee[:], Zs[:, :].unsqueeze(2).to_broadcast([128, 8, 16]), ALU.mult,
                        [Ee, Zs], [GW])
                    for which, (Pf, p_) in enumerate(((PAf, 0), (PBf, 1))):
                        EQ3 = cand[:, :, :].rearrange("p h (k a) -> p (h k) a", a=16)
                        VTT(EQ3, iota_f[:, 0:16].unsqueeze(1).to_broadcast([128, 128, 16]),
                            Pf[:, :].unsqueeze(2).to_broadcast([128, 128, 16]), ALU.is_equal, [iota_f, Pf], [cand])
                        yield
                        EQ4 = cand[:, :, :].rearrange("p h (k a) -> p h k a", a=16)
                        VTT(EQ4, EQ4, If_[:, :, p_, :].unsqueeze(2).to_broadcast([128, 8, 16, 16]), ALU.mult, [If_], [cand])
                        yield
                        S.op("vector", lambda e, which=which, EQ3=EQ3: e.tensor_reduce(out=I12G[which][:], in_=EQ3, axis=AX.X, op=ALU.add),
                             [cand], [I12G[which]])
                        yield
                    if dbg and blk == 0 and tt == 0:
                        for q in range(3):
                            DMA(dbgo[:, q * 128:(q + 1) * 128], I12G[q][:], [I12G[q]], [Buf(None)])
                        DMA(dbgo[:, 384:384 + 128], SC[:, :, :].rearrange("p h k -> p (h k)"), SC_a + SC_b, [Buf(None)])
                        DMA(dbgo[:, 2048:4096], S_sb[:], [S_sb], [Buf(None)])
                    yield
                    MM([(PQ[:, q * 128:(q + 1) * 128], I12G[q][:], ident_f[:], True, True) for q in range(3)], I12G + [ident_f], [PQ])
                    yield
                    for q in range(3):
                        VCOPY(IGT[q][:, tsl], PQ[:, q * 128:(q + 1) * 128], [PQ], [IGT[q]])
                    yield

            def gbuild(blk):
                for t in range(TB):
                    l_, r_ = lt[t % NOH], rt[t % NOH]
                    VSTT(l_[:], iota_b[:], IGT[0][:, t:t + 1], IGT[2][:, t:t + 1].to_broadcast([128, 128]), ALU.is_equal, ALU.mult,
                         [iota_b, IGT[0], IGT[2]], [l_])
                    VTS(r_[:], iota_b[:], IGT[1][:, t:t + 1], None, ALU.is_equal, None, [iota_b, IGT[1]], [r_])
                    pg = PB[4 + (t // 4) % 4]
                    MM([(pg[:, (t % 4) * 128:(t % 4 + 1) * 128], r_[:], l_[:], True, True)], [l_, r_], [pg])
                    if t % 4 == 3:
                        t0 = t - 3
                        ACT(G_all[:, t0:t0 + 4, :], pg[:, :].rearrange("p (t i) -> p t i", i=128), AF.Copy, [pg], [G_all])

            def sweep(blk, filler):
                h2T = h2Tb[blk % 2]
                LOOK = 2
                NAB = 3

                def a_mm(i):
                    ut = UTb[i % NUT]
                    DMA(ut[:], ULb[i].rearrange("p (j e) -> p j e", e=128), UV_parts, [ut])
                    pa = PB[4 + i % NAB]
                    MM([(pa[:, 0:TB], ut[:, j, :], h2T[:, j, :], j == 0, j == 7) for j in range(8)], [ut, h2T], [pa])
                for i in range(LOOK):
                    a_mm(i)
                DMA(Vcb[0][:], Vb[0], UV_parts, [Vcb[0]])
                for i in range(NCH):
                    vc = Vcb[i % NVC]
                    if i + 1 < NCH:
                        DMA(Vcb[(i + 1) % NVC][:], Vb[i + 1], UV_parts, [Vcb[(i + 1) % NVC]])
                    if i + LOOK < NCH:
                        a_mm(i + LOOK)
                    pa = PB[4 + i % NAB]
                    ga = gab[i % NGA]
                    ACT(ga[:], pa[:, 0:TB], AF.Gelu_apprx_tanh, [pa], [ga])
                    wt = Wtb[i % NWT]
                    VTT(wt[:], ga[:], G_all[:, :, i], ALU.mult, [ga, G_all], [wt])
                    outs = []
                    for tt in range(NTT):
                        for dh in range(2):
                            outs.append((PB[tt * 2 + dh][:, :], wt[:, tt * 128:(tt + 1) * 128], vc[:, dh * 512:(dh + 1) * 512], i == 0, i == NCH - 1))
                    MM(outs, [wt, vc], PB[0:2 * NTT])
                    if filler is not None and i >= 2:
                        next(filler, None)

            def epilogue(blk):
                for tt in range(NTT):
                    r0 = blk * TB + tt * 128
                    xb = load_x(X1d[r0:r0 + 128, :])
                    for dh in range(2):
                        pb = PB[tt * 2 + dh]
                        cs = slice(dh * 512, (dh + 1) * 512)
                        VTT(pb[:, :], pb[:, :], g2B[:, cs], ALU.mult, [g2B], [pb])
                        VTT(xb[:, cs], xb[:, cs], pb[:, :], ALU.add, [pb], [xb])
                    r = rms_rstd(xb[:], xb, D)
                    VSTT(xb[:], xb[:], r[:], fngB[:], ALU.mult, ALU.mult, [r, fngB], [xb])
                    part = Buf(None)
                    out_parts.append(part)
                    DMA(out[r0:r0 + 128, :], xb[:], [xb], [part], q="gpsimd")

            for _ in gen_topk(0):
                pass
            for blk in range(NBLK):
                gbuild(blk)
                nxt = gen_topk(blk + 1) if blk + 1 < NBLK else None
                sweep(blk, nxt)
                if nxt is not None:
                    for _ in nxt:
                        pass
                epilogue(blk)
            S.end_phase()
        S.cur_es = es
    return nc


def host_inputs(inputs, core):
    b, s = core // 2, core % 2
    f32 = np.float32
    x = np.asarray(inputs["x"], f32)
    ctx = np.asarray(inputs["ctx"], f32)
    m = {}
    m["xf"] = np.ascontiguousarray(np.concatenate([ctx[b], x[b]], axis=0))
    m["xm"] = np.ascontiguousarray(x[b, s * HALF:(s + 1) * HALF])
    cv = np.stack([np.asarray(inputs["c"], f32)[b], np.asarray(inputs["c_ctx"], f32)], axis=-1)
    m["cvec"] = np.ascontiguousarray(cv.reshape(8, 128, 2).transpose(1, 0, 2))
    m["wmod"] = np.ascontiguousarray(np.asarray(inputs["w_mod"], f32)[0])
    bm = np.asarray(inputs["b_mod"], f32)[0]
    m["bmodT"] = np.ascontiguousarray(bm.reshape(48, 128).T)
    m["bmod"] = np.ascontiguousarray(bm.reshape(1, -1))
    ngs = np.stack([np.asarray(inputs["norm1_g"], f32)[0], np.asarray(inputs["norm2_g"], f32)[0]], axis=-1)
    m["ngT"] = np.ascontiguousarray(ngs.reshape(8, 128, 2).transpose(1, 0, 2))
    m["fng"] = np.ascontiguousarray(np.asarray(inputs["final_norm_g"], f32).reshape(1, -1))
    m["win"] = np.ascontiguousarray(np.asarray(inputs["w_in"], f32)[0])
    cw = np.asarray(inputs["conv_w"], f32)[0]
    m["convw"] = np.ascontiguousarray(cw.reshape(4, 4, 128).transpose(2, 1, 0))
    m["convb"] = np.ascontiguousarray(np.asarray(inputs["conv_b"], f32)[0].reshape(4, 128).T)
    wa = np.asarray(inputs["lru_w_a"], f32)[0]
    wx = np.asarray(inputs["lru_w_x"], f32)[0]
    lw = np.zeros((2, 2, 4, 128, 128), f32)
    for d in range(2):
        for g, w in enumerate((wa, wx)):
            for ct in range(4):
                lw[d, g, ct, 0:64, 0:64] = w[d, 2 * ct]
                lw[d, g, ct, 64:128, 64:128] = w[d, 2 * ct + 1]
    m["lruw"] = lw
    ba = np.asarray(inputs["lru_b_a"], f32)[0]
    bx = np.asarray(inputs["lru_b_x"], f32)[0]
    lb = np.stack([ba, bx], axis=1)
    m["lrub"] = np.ascontiguousarray(lb.reshape(2, 2, 4, 128).transpose(3, 0, 1, 2))
    lam = np.asarray(inputs["lru_lambda"], f32)[0]
    m["lrulam"] = np.ascontiguousarray(lam.reshape(2, 4, 128).transpose(2, 0, 1))
    gf = np.asarray(inputs["fourier_out_g"], f32)[0].reshape(4, 128).T
    gr = np.asarray(inputs["lru_out_g"], f32)[0].reshape(4, 128).T
    m["gFR"] = np.ascontiguousarray(np.concatenate([gf, gr], axis=1))
    m["wout"] = np.ascontiguousarray(np.asarray(inputs["w_out"], f32)[0])
    m["wq"] = np.ascontiguousarray(np.asarray(inputs["peer_w_q"], f32)[0])
    sk = np.asarray(inputs["peer_sub_keys"], f32)[0]
    m["keysT"] = np.ascontiguousarray(sk.reshape(16, 128, 128).transpose(2, 0, 1))
    u = np.asarray(inputs["peer_u"], f32)[0]
    m["UL"] = np.ascontiguousarray(u.reshape(128, 128, 8, 128).transpose(0, 3, 2, 1).reshape(128, 128, 1024))
    m["Vt"] = np.ascontiguousarray(np.asarray(inputs["peer_v"], f32)[0].reshape(128, 128, 1024))
    n1 = np.arange(128, dtype=np.float64)[:, None, None]
    n2 = np.arange(64, dtype=np.float64)[None, :, None]
    k1 = np.arange(128, dtype=np.float64)[None, None, :]
    ph = 2 * np.pi * (k1 * n1 / 128.0 + k1 * n2 / 8192.0)
    m["watab"] = np.ascontiguousarray(np.stack([np.cos(ph), -np.sin(ph)], axis=2).astype(f32))
    k2 = (32 * s + np.arange(32, dtype=np.float64))[None, :]
    nn = np.arange(64, dtype=np.float64)[:, None]
    ph2 = 2 * np.pi * k2 * nn / 64.0
    m["c64s"] = np.ascontiguousarray(np.stack([np.cos(ph2), np.sin(ph2), -np.sin(ph2)], axis=1).astype(f32))
    jj = np.arange(64, dtype=np.float64)[:, None]
    mm = np.arange(64, dtype=np.float64)[None, :]
    sc = 1.0 / np.sqrt(8192.0 * 64.0)
    c6 = np.cos(2 * np.pi * jj * mm / 64.0) * sc
    s6 = np.sin(2 * np.pi * jj * mm / 64.0) * sc
    bd = np.zeros((128, 2, 128))
    bd[0:64, 0, 0:64] = c6
    bd[64:128, 0, 64:128] = c6
    bd[0:64, 1, 0:64] = s6
    bd[64:128, 1, 64:128] = s6
    m["bd64"] = np.ascontiguousarray(bd.astype(f32))
    se = np.zeros((128, 2), f32)
    se[:, s] = 1.0
    m["sel"] = se
    return m


_NC_CACHE = {}


def kernel(**inputs):
    if "nc" not in _NC_CACHE:
        _NC_CACHE["nc"] = build()
    nc = _NC_CACHE["nc"]
    in_maps = [host_inputs(inputs, c) for c in range(8)]
    res = run_bass_kernel_spmd(nc, in_maps, core_ids=list(range(8)))
    outp = np.zeros((4, L, D), np.float32)
    for c in range(8):
        b, s = c // 2, c % 2
        outp[b, s * HALF:(s + 1) * HALF] = res.results[c]["out"]
    return outp
```
